# Optimizing a Trainium2 kernel written in Bass

```python
import jax, jax.numpy as jnp
from jax import lax
import numpy as np

D_MODEL = 1024
BATCH = 4
SEQ = 4096
DEPTH = 2

GLA_HEADS = 4
GLA_DK = 48
GLA_DV = 96
GLA_LOWRANK = 16
GLA_GATE_NORMALIZER = 16.0
SB_HEADS = 6
SB_DH = 64
SB_BLOCK = 128
HG_HEADS = 4
HG_DK = 128
HG_DV = 64
CHUNK = 16

MIX_WIDTH = GLA_HEADS * GLA_DV + SB_HEADS * SB_DH + HG_HEADS * HG_DV
D_FF = -(-(8 * D_MODEL) // (3 * 256)) * 256
RMS_EPS = 1e-6

IN_SIZES = (
    GLA_HEADS * GLA_DK,
    GLA_HEADS * GLA_DK,
    GLA_HEADS * GLA_DV,
    GLA_LOWRANK,
    GLA_HEADS * GLA_DV,
    SB_HEADS * SB_DH,
    SB_HEADS * SB_DH,
    SB_HEADS * SB_DH,
    HG_HEADS * HG_DK,
    HG_HEADS * HG_DK,
    HG_HEADS * HG_DV,
    HG_HEADS * HG_DV,
)
IN_COLS = int(sum(IN_SIZES))
SPLIT_IDX = tuple(int(c) for c in np.cumsum(IN_SIZES)[:-1])

kernel_name = "hybrid_gla_stickbreak_hgrn2_block"


def rms_norm(x, g):
    xf = x.astype(jnp.float32)
    y = xf * lax.rsqrt(jnp.mean(xf * xf, axis=-1, keepdims=True) + RMS_EPS)
    return (y * g.astype(jnp.float32)).astype(x.dtype)


def split_heads(t, n_heads):
    b, s, _ = t.shape
    return t.reshape(b, s, n_heads, -1).transpose(0, 2, 1, 3)


def merge_heads(t):
    b, h, s, d = t.shape
    return t.transpose(0, 2, 1, 3).reshape(b, s, h * d)


def chunked_gated_linear_attention(q, k, v, log_f):
    b, h, s, dk = q.shape
    dv = v.shape[-1]
    n = s // CHUNK
    q = q.reshape(b, h, n, CHUNK, dk)
    k = k.reshape(b, h, n, CHUNK, dk)
    v = v.reshape(b, h, n, CHUNK, dv)
    g_cum = jnp.cumsum(log_f.reshape(b, h, n, CHUNK, dk), axis=3)
    g_last = g_cum[:, :, :, -1:, :]
    q_dec = q * jnp.exp(g_cum)
    k_to_end = k * jnp.exp(g_last - g_cum)
    causal = jnp.tril(jnp.ones((CHUNK, CHUNK), dtype=bool))[:, :, None]
    rel = g_cum[:, :, :, :, None, :] - g_cum[:, :, :, None, :, :]
    rel_decay = jnp.exp(jnp.where(causal, rel, -jnp.inf))
    scores = jnp.einsum('bhnik,bhnjk,bhnijk->bhnij', q, k, rel_decay)
    o_intra = jnp.einsum('bhnij,bhnjv->bhniv', scores, v)
    chunk_states = jnp.einsum('bhnjk,bhnjv->bhnkv', k_to_end, v)
    chunk_decay = jnp.exp(g_last[:, :, :, 0, :])

    def step(state, inp):
        dec, upd = inp
        return dec[..., None] * state + upd, state

    init = jnp.zeros((b, h, dk, dv), jnp.float32)
    _, prev_states = lax.scan(step, init, (jnp.moveaxis(chunk_decay, 2, 0),
                                           jnp.moveaxis(chunk_states, 2, 0)))
    prev_states = jnp.moveaxis(prev_states, 0, 2)
    o_inter = jnp.einsum('bhnik,bhnkv->bhniv', q_dec, prev_states)
    return (o_intra + o_inter).reshape(b, h, s, dv)


def stick_breaking_attention(q, k, v):
    b, h, s, d = q.shape
    scale = d ** -0.5
    outs = []
    for start in range(0, s, SB_BLOCK):
        end = start + SB_BLOCK
        qb = q[:, :, start:end]
        kb = k[:, :, :end]
        vb = v[:, :, :end]
        z = jnp.einsum('bhqd,bhkd->bhqk', qb, kb) * scale
        t_pos = start + jnp.arange(SB_BLOCK)
        s_pos = jnp.arange(end)
        mask = s_pos[None, :] < t_pos[:, None]
        log_keep = jnp.where(mask, jax.nn.log_sigmoid(-z), 0.0)
        log_rest = lax.cumsum(log_keep, axis=3, reverse=True) - log_keep
        log_w = jnp.where(mask, jax.nn.log_sigmoid(z) + log_rest, -jnp.inf)
        weights = jnp.exp(log_w)
        outs.append(jnp.einsum('bhqk,bhkd->bhqd', weights, vb))
    return jnp.concatenate(outs, axis=2)


def setup_inputs(seed: int = 0) -> dict:
    key = jax.random.key(seed)
    ks = jax.random.split(key, 16)
    f32 = jnp.float32

    def gain(k_, shape):
        return 1.0 + 0.02 * jax.random.normal(k_, shape, f32)

    return {
        "x": jax.random.normal(ks[0], (BATCH, SEQ, D_MODEL), f32),
        "norm_mix_g": gain(ks[1], (DEPTH, D_MODEL)),
        "w_in": jax.random.normal(ks[2], (DEPTH, D_MODEL, IN_COLS), f32) * D_MODEL ** -0.5,
        "gla_w_decay": jax.random.normal(ks[3], (DEPTH, GLA_LOWRANK, GLA_HEADS * GLA_DK), f32) * GLA_LOWRANK ** -0.5,
        "gla_b_decay": 0.01 * jax.random.normal(ks[4], (DEPTH, GLA_HEADS * GLA_DK), f32),
        "gla_out_g": gain(ks[5], (DEPTH, GLA_DV)),
        "sb_q_g": gain(ks[6], (DEPTH, SB_DH)),
        "sb_k_g": gain(ks[7], (DEPTH, SB_DH)),
        "sb_out_g": gain(ks[8], (DEPTH, SB_DH)),
        "hg_out_g": gain(ks[9], (DEPTH, HG_DV)),
        "hg_lb_logits": 0.1 * jax.random.normal(ks[10], (DEPTH, HG_HEADS * HG_DK), f32),
        "w_out": jax.random.normal(ks[11], (DEPTH, MIX_WIDTH, D_MODEL), f32) * MIX_WIDTH ** -0.5,
        "norm_ffn_g": gain(ks[12], (DEPTH, D_MODEL)),
        "w_ffn_up": jax.random.normal(ks[13], (DEPTH, D_MODEL, 2 * D_FF), f32) * D_MODEL ** -0.5,
        "w_ffn_down": jax.random.normal(ks[14], (DEPTH, D_FF, D_MODEL), f32) * D_FF ** -0.5,
    }


def reference(x, norm_mix_g, w_in, gla_w_decay, gla_b_decay, gla_out_g, sb_q_g,
              sb_k_g, sb_out_g, hg_out_g, hg_lb_logits, w_out, norm_ffn_g,
              w_ffn_up, w_ffn_down):
    f32 = jnp.float32
    lb_probs = jax.nn.softmax(hg_lb_logits.astype(f32), axis=0)
    lower_bounds = jnp.cumsum(lb_probs, axis=0) - lb_probs[0:1]

    for li in range(DEPTH):
        h = rms_norm(x, norm_mix_g[li])
        proj = h @ w_in[li]
        (gq, gk, gv, g_lr, g_gate, sq, sk, sv,
         hq, hf, hi, h_gate) = jnp.split(proj.astype(f32), SPLIT_IDX, axis=-1)

        log_alpha = jax.nn.log_sigmoid(g_lr @ gla_w_decay[li].astype(f32)
                                       + gla_b_decay[li].astype(f32)) / GLA_GATE_NORMALIZER
        o_a = chunked_gated_linear_attention(
            split_heads(gq, GLA_HEADS) * GLA_DK ** -0.5,
            split_heads(gk, GLA_HEADS),
            split_heads(gv, GLA_HEADS),
            split_heads(log_alpha, GLA_HEADS))
        o_a = merge_heads(rms_norm(o_a, gla_out_g[li])) * jax.nn.silu(g_gate)

        q_b = rms_norm(split_heads(sq, SB_HEADS), sb_q_g[li])
        k_b = rms_norm(split_heads(sk, SB_HEADS), sb_k_g[li])
        o_b = stick_breaking_attention(q_b, k_b, split_heads(sv, SB_HEADS))
        o_b = merge_heads(rms_norm(o_b, sb_out_g[li]))

        lb = lower_bounds[li]
        log_sig = jax.nn.log_sigmoid(hf)
        log_lb = jnp.log(jnp.maximum(lb, 1e-30))
        log_f = jnp.where(lb > 0.0,
                          jnp.logaddexp(log_lb, jnp.log1p(-lb) + log_sig),
                          log_sig)
        k_c = -jnp.expm1(log_f)
        o_c = chunked_gated_linear_attention(
            split_heads(hq, HG_HEADS),
            split_heads(k_c, HG_HEADS),
            split_heads(hi, HG_HEADS),
            split_heads(log_f, HG_HEADS))
        o_c = merge_heads(rms_norm(o_c, hg_out_g[li])) * jax.nn.silu(h_gate)

        mixed = jnp.concatenate([o_a, o_b, o_c], axis=-1).astype(x.dtype)
        x = x + mixed @ w_out[li]

        hn = rms_norm(x, norm_ffn_g[li])
        gate, up = jnp.split(hn @ w_ffn_up[li], 2, axis=-1)
        x = x + (jax.nn.silu(gate) * up) @ w_ffn_down[li]
    return x
```

```python
import numpy as np
import ml_dtypes
import concourse.bass as bass
import concourse.mybir as mybir
from concourse.bass_utils import run_bass_kernel_spmd

F32 = mybir.dt.float32
BF16 = mybir.dt.bfloat16
AF = mybir.ActivationFunctionType
ALU = mybir.AluOpType
AX = mybir.AxisListType
NPBF = ml_dtypes.bfloat16

D = 1024
DFF = 2816
EPS = 1e-6


class Prog:
    ENGS = ("pe", "act", "dve", "pool", "sp")

    PHASE = [0]

    def __init__(self, nc, same_eng_sync=True):
        self.nc = nc
        Prog.PHASE[0] += 1
        self.pfx = f"p{Prog.PHASE[0]}_"
        self.q = {e: [] for e in self.ENGS}
        self.cnt = {}
        self.last_w = {}
        self.readers = {}
        self.waited = {e: {} for e in self.ENGS}
        self.same = same_eng_sync
        self.sem_names = set(self.ENGS)
        self.n_ops = 0

    def _deps(self, eng, reads, writes, own_sem):
        need = {}

        def add(sv):
            if sv is None:
                return
            s, v = sv
            if s == own_sem and (not self.same or eng == "pe"):
                return
            if need.get(s, 0) < v:
                need[s] = v

        for k in reads:
            add(self.last_w.get(k))
        for k in writes:
            add(self.last_w.get(k))
            for s, v in self.readers.get(k, {}).items():
                add((s, v))
        out = []
        for s, v in need.items():
            if self.waited[eng].get(s, 0) < v:
                self.waited[eng][s] = v
                out.append((s, v))
        return out

    def _mark(self, reads, writes, sem, val):
        for k in reads:
            self.readers.setdefault(k, {})[sem] = val
        for k in writes:
            self.last_w[k] = (sem, val)
            self.readers[k] = {}

    def op(self, eng, fn, reads=(), writes=()):
        import os
        if self.n_ops >= int(os.environ.get("PROG_MAXOPS", "100000000")):
            return
        waits = self._deps(eng, reads, writes, eng)
        v = self.cnt.get(eng, 0) + 1
        self.cnt[eng] = v
        self.q[eng].append((waits, fn, eng, 1))
        self._mark(reads, writes, eng, v)
        self.n_ops += 1

    def dma(self, queue, sem, pairs, reads=(), writes=(), **kw):
        import os
        if self.n_ops >= int(os.environ.get("PROG_MAXOPS", "100000000")):
            return
        self.n_ops += 1
        self.sem_names.add(sem)
        waits = self._deps(queue, reads, writes, None)
        base = self.cnt.get(sem, 0)
        for i, (o, i_) in enumerate(pairs):
            def fn(e, o=o, i_=i_):
                return e.dma_start(out=o, in_=i_, **kw)
            self.q[queue].append((waits if i == 0 else [], fn, sem, 16))
        v = base + 16 * len(pairs)
        self.cnt[sem] = v
        self._mark(reads, writes, sem, v)

    def wait_all(self, eng, keys):
        waits = self._deps(eng, keys, (), None)
        import os
        if os.environ.get("PROG_MAXOPS"):
            waits = [(s_, v) for s_, v in self.cnt.items() if v > 0]
        self.q[eng].append((waits, None, None, 0))

    def emit(self):
        nc = self.nc
        import contextlib
        with contextlib.ExitStack() as st:
            sems = {n: st.enter_context(nc.semaphore(self.pfx + "s_" + n)) for n in sorted(self.sem_names)}
            block = st.enter_context(nc.Block())

            def replay(name):
                def run(e):
                    for waits, fn, sem, inc in self.q[name]:
                        for s, v in waits:
                            e.wait_ge(sems[s], v)
                        if fn is not None:
                            ins = fn(e)
                            ins.then_inc(sems[sem], inc)
                return run

            block.sync(replay("sp"))
            block.tensor(replay("pe"))
            block.scalar(replay("act"))
            block.vector(replay("dve"))
            block.gpsimd(replay("pool"))


class Alloc:
    CNT = [0]

    def __init__(self, nc):
        import contextlib
        self.nc = nc
        self.st = contextlib.ExitStack()
        Alloc.CNT[0] += 1
        self.pfx = f"a{Alloc.CNT[0]}_"

    def sb(self, name, shape, dt):
        return self.st.enter_context(self.nc.sbuf_tensor(self.pfx + "sb_" + name, list(shape), dt))

    def ps(self, name, shape, dt):
        return self.st.enter_context(self.nc.psum_tensor(self.pfx + "ps_" + name, list(shape), dt))

    def close(self):
        self.st.close()


FF_GROUPS = [(0, 4), (4, 4), (8, 4), (12, 4), (16, 3), (19, 3)]
UNIT = 1024


def build_ffn(NT):
    assert NT % UNIT == 0 or NT == 512
    unit = min(UNIT, NT)
    n_units = NT // unit
    nc = bass.Bass("TRN2", target_bir_lowering=False)
    x = nc.dram_tensor("x", [NT, D], F32, kind="ExternalInput").ap()
    mT = nc.dram_tensor("mT", [D, NT], BF16, kind="ExternalInput").ap()
    w_out = nc.dram_tensor("w_out", [D, D], F32, kind="ExternalInput").ap()
    w_up = nc.dram_tensor("w_up", [D, 2 * DFF], F32, kind="ExternalInput").ap()
    w_down = nc.dram_tensor("w_down", [DFF, D], F32, kind="ExternalInput").ap()
    g_bc = nc.dram_tensor("g_bc", [128, D], F32, kind="ExternalInput").ap()
    ident = nc.dram_tensor("ident", [128, 128], BF16, kind="ExternalInput").ap()
    y = nc.dram_tensor("y", [NT, D], F32, kind="ExternalOutput").ap()

    A = Alloc(nc)
    P = Prog(nc)
    emit_ffn(nc, A, P, x, mT, w_out, w_up, w_down, g_bc, ident, y, NT, unit, n_units)
    P.wait_all("sp", [("y", u) for u in range(n_units)])
    P.emit()
    A.close()
    return nc


def emit_ffn(nc, A, P, x, mT, w_out, w_up, w_down, g_bc, ident, y, NT, unit, n_units):
    ntile = unit // 512
    nblk = unit // 128
    wout_sb = A.sb("wout_sb", [128, 8, D], BF16)
    stage = [A.sb(f"stage{i}", [128, 2048], F32) for i in range(2)]
    xres = A.sb("xres", [128, nblk, D], F32)
    hnT = A.sb("hnT", [128, 8, unit], BF16)
    mts = [A.sb(f"mts{i}", [128, 8, 512], BF16) for i in range(2)]
    wup = [A.sb(f"wup{i}", [128, 8, 2, 512], BF16) for i in range(2)]
    wdn = [A.sb(f"wdn{i}", [128, 4, D], BF16) for i in range(2)]
    actT = [A.sb(f"actT{i}", [128, 4, 512], BF16) for i in range(2)]
    sg = [A.sb(f"sg{i}", [128, 512], F32) for i in range(2)]
    gbc = A.sb("gbc", [128, D], F32)
    idn = A.sb("idn", [128, 128], BF16)
    hn = [A.sb(f"hn{i}", [128, D], BF16) for i in range(2)]
    sq = A.sb("sq", [128, D], BF16)
    stat = A.sb("stat", [128, 64], F32)
    pb = [A.ps(f"pb{i}", [128, 512], F32) for i in range(7)]
    ptr = A.ps("ptr", [128, 8, 128], BF16)

    st_i = [0]

    def load_cast(dst_ap, src_ap, shape, dkey):
        i = st_i[0] % 2
        st_i[0] += 1
        n = int(np.prod(shape))
        sv = stage[i][:, 0:n]
        if len(shape) == 2:
            sv = sv.rearrange("p (a b) -> p a b", a=shape[0])
        P.dma("sp", f"stg{i}", [(sv, src_ap)], writes=[("stage", i)])
        if i == 0:
            P.op("pool", lambda e: e.tensor_copy(out=dst_ap, in_=sv),
                 reads=[("stage", i)], writes=[dkey])
        else:
            P.op("act", lambda e: e.activation(out=dst_ap, in_=sv, func=AF.Copy),
                 reads=[("stage", i)], writes=[dkey])

    P.dma("sp", "const", [(gbc[:], g_bc), (idn[:], ident)], writes=["gbc", "idn"])
    for c2 in range(4):
        load_cast(wout_sb[:, 2 * c2:2 * c2 + 2, :],
                  w_out[c2 * 256:(c2 + 1) * 256, :].rearrange("(c p) n -> p c n", p=128),
                  (2, D), ("wout", c2))

    pbi = [0]

    def next_pb(lo, hi):
        i = lo + pbi[0] % (hi - lo)
        pbi[0] += 1
        return i

    for u in range(n_units):
        t0 = u * unit
        for t in range(ntile):
            tt = t0 + t * 512
            mi = (u * ntile + t) % 2
            P.dma("sp", f"mts{mi}", [(mts[mi][:], mT[:, tt:tt + 512].rearrange("(c p) t -> p c t", p=128))],
                  writes=[("mts", mi)])
            xv = xres[:, 4 * t:4 * t + 4, :]
            P.dma("sp", f"xin{t % 2}", [(xv, x[tt:tt + 512, :].rearrange("(b p) d -> p b d", p=128))],
                  writes=[("xres", 4 * t + b) for b in range(4)])
            for b in range(4):
                bb = 4 * t + b
                for h in range(2):
                    k = next_pb(0, 4)
                    for c in range(8):
                        P.op("pe", lambda e, k=k, c=c, b=b, h=h, mi=mi: e.matmul(
                            pb[k][:], lhsT=mts[mi][:, c, b * 128:(b + 1) * 128],
                            rhs=wout_sb[:, c, h * 512:(h + 1) * 512], start=(c == 0), stop=(c == 7)),
                            reads=[("mts", mi), ("wout", c // 2)], writes=[("pb", k)])
                    P.op("dve", lambda e, k=k, bb=bb, h=h: e.tensor_tensor(
                        out=xres[:, bb, h * 512:(h + 1) * 512], in0=pb[k][:],
                        in1=xres[:, bb, h * 512:(h + 1) * 512], op=ALU.add),
                        reads=[("pb", k)], writes=[("xres", bb)])
                P.op("act", lambda e, bb=bb: e.activation(out=sq[:], in_=xres[:, bb, :], func=AF.Square,
                                                          accum_out=stat[:, bb:bb + 1]),
                     reads=[("xres", bb)], writes=["sq", ("stat", bb)])
                P.op("act", lambda e, bb=bb: e.activation(out=stat[:, 16 + bb:17 + bb], in_=stat[:, bb:bb + 1],
                                                          func=AF.Ln, scale=1.0 / D, bias=EPS),
                     reads=[("stat", bb)], writes=[("stat2", bb)])
                P.op("act", lambda e, bb=bb: e.activation(out=stat[:, 32 + bb:33 + bb], in_=stat[:, 16 + bb:17 + bb],
                                                          func=AF.Exp, scale=-0.5),
                     reads=[("stat2", bb)], writes=[("stat3", bb)])
                hi = bb % 2
                P.op("dve", lambda e, bb=bb, hi=hi: e.scalar_tensor_tensor(
                    out=hn[hi][:], in0=xres[:, bb, :], scalar=stat[:, 32 + bb:33 + bb], in1=gbc[:],
                    op0=ALU.mult, op1=ALU.mult),
                    reads=[("xres", bb), ("stat3", bb), "gbc"], writes=[("hn", hi)])
                for c in range(8):
                    P.op("pe", lambda e, c=c, hi=hi: e.transpose(ptr[:, c, :], hn[hi][:, c * 128:(c + 1) * 128], idn[:]),
                         reads=[("hn", hi), "idn"], writes=["ptr"])
                P.op("act", lambda e, bb=bb: e.activation(out=hnT[:, :, bb * 128:(bb + 1) * 128], in_=ptr[:],
                                                          func=AF.Copy),
                     reads=["ptr"], writes=[("hnT", bb)])
        for gi, (j0, G) in enumerate(FF_GROUPS):
            wi = (u * len(FF_GROUPS) + gi) % 2
            gw = G * 128
            for s in range(2):
                for ch in range(2):
                    c0 = s * DFF + j0 * 128
                    load_cast(wup[wi][:, 4 * ch:4 * ch + 4, s, 0:gw],
                              w_up[ch * 512:(ch + 1) * 512, c0:c0 + gw].rearrange("(c p) n -> p c n", p=128),
                              (4, gw), ("wup", wi, s, ch))
            for jh in range(0, G, 2):
                n = min(2, G - jh)
                load_cast(wdn[wi][:, jh:jh + n, :],
                          w_down[(j0 + jh) * 128:(j0 + jh + n) * 128, :].rearrange("(j p) n -> p j n", p=128),
                          (n, D), ("wdn", wi, jh // 2))
            for t in range(ntile):
                ai = (gi * ntile + t) % 2
                for j in range(G):
                    kg = next_pb(0, 4)
                    ku = next_pb(0, 4)
                    for s, k in ((0, kg), (1, ku)):
                        for c in range(8):
                            P.op("pe", lambda e, k=k, c=c, s=s, j=j, t=t, wi=wi: e.matmul(
                                pb[k][:], lhsT=wup[wi][:, c, s, j * 128:(j + 1) * 128],
                                rhs=hnT[:, c, t * 512:(t + 1) * 512], start=(c == 0), stop=(c == 7)),
                                reads=[("wup", wi, s, c // 4)] + [("hnT", 4 * t + b) for b in range(4)],
                                writes=[("pb", k)])
                    si = j % 2
                    P.op("act", lambda e, kg=kg, si=si: e.activation(out=sg[si][:], in_=pb[kg][:], func=AF.Silu),
                         reads=[("pb", kg)], writes=[("sg", si)])
                    P.op("dve", lambda e, ku=ku, si=si, ai=ai, j=j: e.tensor_tensor(
                        out=actT[ai][:, j, :], in0=pb[ku][:], in1=sg[si][:], op=ALU.mult),
                        reads=[("pb", ku), ("sg", si)], writes=[("actT", ai, j)])
                for b in range(4):
                    bb = 4 * t + b
                    for h in range(2):
                        k = 4 + next_pb(0, 3)
                        for j in range(G):
                            P.op("pe", lambda e, k=k, j=j, b=b, h=h, ai=ai, wi=wi, G=G: e.matmul(
                                pb[k][:], lhsT=actT[ai][:, j, b * 128:(b + 1) * 128],
                                rhs=wdn[wi][:, j, h * 512:(h + 1) * 512], start=(j == 0), stop=(j == G - 1)),
                                reads=[("actT", ai, j), ("wdn", wi, j // 2)], writes=[("pb", k)])
                        P.op("dve", lambda e, k=k, bb=bb, h=h: e.tensor_tensor(
                            out=xres[:, bb, h * 512:(h + 1) * 512], in0=pb[k][:],
                            in1=xres[:, bb, h * 512:(h + 1) * 512], op=ALU.add),
                            reads=[("pb", k)], writes=[("xres", bb)])
        P.dma("sp", "yout", [(y[t0:t0 + unit, :].rearrange("(b p) d -> p b d", p=128), xres[:])],
              reads=[("xres", bb) for bb in range(nblk)], writes=[("y", u)])


FM = [("gq", 128), ("gk", 128), ("glr", 16), ("gg0", 96), ("gg1", 96),
      ("sq0", 64), ("sq1", 64), ("sq2", 64), ("sk0", 64), ("sk1", 64), ("sk2", 64),
      ("hq0", 128), ("hq1", 128), ("hf0", 128), ("hf1", 128), ("hgt0", 64), ("hgt1", 64)]
FM_OFF = {}
_o = 0
for _n, _m in FM:
    FM_OFF[_n] = (_o, _m)
    _o += ((_m + 63) // 64) * 64
TM_OFF = _o
WC = _o + 512
CV_BDEC, CV_GLAG, CV_SBQ, CV_SBK, CV_SBO, CV_HGO, CV_LB = 0, 1, 2, 3, 4, 5, 6
CF_R128, CF_R64, CF_TRI, CF_BD64, CF_STRICT = 0, 512, 1024, 1152, 1280
CF_M0, CF_M1, CF_P0, CF_P1 = 1408, 1920, 2432, 2433
CF_W = 2436
CB_NEGINCL, CB_NEGONES, CB_ONES, CB_ZERO, CB_IDENT = 0, 128, 256, 384, 512
CB_W = 640


def make_consts():
    f = np.zeros((128, CF_W), np.float32)
    r = np.ones(512, np.float32); r[::128] = 0; f[:, CF_R128:CF_R128 + 512] = r
    r = np.ones(512, np.float32); r[::64] = 0; f[:, CF_R64:CF_R64 + 512] = r
    j = np.arange(128)[:, None]; i = np.arange(128)[None, :]
    f[:, CF_TRI:CF_TRI + 128] = (j <= i)
    f[:, CF_BD64:CF_BD64 + 128] = (j <= i) & ((j // 64) == (i // 64))
    f[:, CF_STRICT:CF_STRICT + 128] = (j < i)
    cc = np.arange(512)
    f[:, CF_M0:CF_M0 + 512] = ((cc % 128) < 64)
    f[:, CF_M1:CF_M1 + 512] = ((cc % 128) >= 64)
    f[0:64, CF_P0] = 1
    f[64:128, CF_P1] = 1
    b = np.zeros((128, CB_W), np.float32)
    b[:, CB_NEGINCL:CB_NEGINCL + 128] = -(j >= i).astype(np.float32)
    b[:, CB_NEGONES:CB_NEGONES + 128] = -1
    b[:, CB_ONES:CB_ONES + 128] = 1
    b[:, CB_IDENT:CB_IDENT + 128] = np.eye(128)
    return f, b.astype(NPBF)


def build_mixer(T, li, parts="sgh"):
    nc = bass.Bass("TRN2", target_bir_lowering=False)
    dr = {}
    dr["x"] = nc.dram_tensor("x", [T, D], F32, kind="ExternalInput").ap()
    dr["w_in"] = nc.dram_tensor("w_in", [D, WC], F32, kind="ExternalInput").ap()
    dr["g_bc"] = nc.dram_tensor("g_bc", [128, D], F32, kind="ExternalInput").ap()
    dr["cvec"] = nc.dram_tensor("cvec", [128, 16], F32, kind="ExternalInput").ap()
    dr["wdec"] = nc.dram_tensor("wdec", [16, 128], F32, kind="ExternalInput").ap()
    dr["cf"] = nc.dram_tensor("cf", [128, CF_W], F32, kind="ExternalInput").ap()
    dr["cb"] = nc.dram_tensor("cb", [128, CB_W], BF16, kind="ExternalInput").ap()
    dr["mT"] = nc.dram_tensor("mT", [512, T], BF16, kind="ExternalOutput").ap()
    A = Alloc(nc)
    P = Prog(nc)
    emit_mixer(nc, A, P, dr, T, li, parts)
    P.wait_all("sp", [("mT", t) for t in range(T // 512)])
    P.emit()
    A.close()
    return nc


def emit_mixer(nc, A, P, dr, T, li, parts):
    NTILE = T // 512
    NBLK = T // 128
    win = A.sb("win", [128, 8, WC], BF16)
    stage = [A.sb(f"stage{i}", [128, 1024], F32) for i in range(2)]
    gbc = A.sb("gbc", [128, D], F32)
    cv = A.sb("cv", [128, 16], F32)
    cv2 = A.sb("cv2", [128, 16], F32)
    wdec_f = A.sb("wdec_f", [16, 128], F32)
    wdec = A.sb("wdec", [16, 128], BF16)
    cf = A.sb("cf", [128, CF_W], F32)
    cb = A.sb("cb", [128, CB_W], BF16)
    xt = A.sb("xt", [128, 4, D], F32)
    sq = A.sb("sq", [128, D], BF16)
    stat = A.sb("stat", [128, 16], F32)
    hn = [A.sb(f"hn{i}", [128, D], BF16) for i in range(2)]
    hT = A.sb("hT", [128, 8, 512], BF16)
    KT = A.sb("KT", [64, 3, T], BF16)
    Vall = A.sb("Vall", [128, NBLK, 192], BF16)
    vt = A.sb("vt", [128, 4, 320], BF16)
    qT = A.sb("qT", [64, 3, 512], BF16)
    rawf = [A.sb(f"rawf{i}", [128, 512], F32) for i in range(2)]
    sqb = A.sb("sqb", [128, 512], BF16)
    lnv = A.sb("lnv", [128, 512], F32)
    rstd = A.sb("rstd", [128, 512], F32)
    ef = [A.sb(f"ef{i}", [128, 512], F32) for i in range(2)]
    spb = [A.sb(f"spb{i}", [128, 512], BF16) for i in range(2)]
    Sb = A.sb("Sb", [128, 512], BF16)
    wg = [A.sb(f"wg{i}", [128, 512], BF16) for i in range(2)]
    sgl = [A.sb(f"sgl{i}", [96, 512], F32) for i in range(2)]
    shg = [A.sb(f"shg{i}", [64, 512], F32) for i in range(2)]
    qg = A.sb("qg", [128, 512], F32)
    kg = A.sb("kg", [128, 512], F32)
    glr = A.sb("glr", [16, 512], BF16)
    qh = [A.sb(f"qh{i}", [128, 512], F32) for i in range(2)]
    kh = [A.sb(f"kh{i}", [128, 512], F32) for i in range(2)]
    lf = A.sb("lf", [128, 512], F32)
    hfa = A.sb("hfa", [128, 512], F32)
    hfb = A.sb("hfb", [128, 512], F32)
    Gs = A.sb("Gs", [128, 512], F32)
    dlt = A.sb("dlt", [128, 512], F32)
    E1 = A.sb("E1", [128, 512], F32)
    E2 = A.sb("E2", [128, 512], F32)
    dl = A.sb("dl", [128, 8], F32)
    qd = A.sb("qd", [128, 512], BF16)
    kd = A.sb("kd", [128, 512], BF16)
    ATs = [A.sb(f"ATs{i}", [128, 128], BF16) for i in range(2)]
    qz = [A.sb(f"qz{i}", [128, 512], BF16) for i in range(2)]
    vz = [A.sb(f"vz{i}", [128, 64], BF16) for i in range(2)]
    ktok = [A.sb(f"ktok{i}", [128, 128], BF16) for i in range(2)]
    Sg = A.sb("Sg", [128, 96], F32)
    Sgb = A.sb("Sgb", [128, 96], BF16)
    Sh = [A.sb(f"Sh{i}", [128, 64], F32) for i in range(2)]
    Shb = [A.sb(f"Shb{i}", [128, 64], BF16) for i in range(2)]
    onrm = A.sb("onrm", [128, 512], F32)
    mo = [A.sb("mo0", [128, 7, 512], BF16)] * 2
    pb = [A.ps(f"pb{i}", [128, 512], F32) for i in range(7)]
    ptr = A.ps("ptr", [128, 8, 128], BF16)

    def ones(m):
        return cb[0:m, CB_ONES:CB_ONES + m]

    idn_t = A.sb("idn_t", [128, 128], BF16)
    idn = idn_t[:]

    st_i = [0]

    def load_cast(dst_ap, src_ap, shape, dkey):
        i = st_i[0] % 2
        st_i[0] += 1
        n = int(np.prod(shape))
        sv = stage[i][:, 0:n]
        if len(shape) == 2:
            sv = sv.rearrange("p (a b) -> p a b", a=shape[0])
        P.dma("sp", f"stg{i}", [(sv, src_ap)], writes=[("stage", i)])
        P.op("pool", lambda e: e.tensor_copy(out=dst_ap, in_=sv), reads=[("stage", i)], writes=[dkey])

    P.dma("sp", "const", [(gbc[:], dr["g_bc"]), (cv[:], dr["cvec"]), (wdec_f[:], dr["wdec"]),
                          (cf[:], dr["cf"]), (cb[:], dr["cb"]), (idn_t[:], dr["cb"][:, CB_IDENT:CB_IDENT + 128])],
          writes=["gbc", "cv", "wdec_f", "cf", "cb"])
    for c in range(8):
        for hf in range(3):
            load_cast(win[:, c, hf * 704:(hf + 1) * 704], dr["w_in"][c * 128:(c + 1) * 128, hf * 704:(hf + 1) * 704],
                      (704,), ("win", c))
    P.op("dve", lambda e: e.tensor_copy(out=wdec[:], in_=wdec_f[:]), reads=["wdec_f"], writes=["wdec"])
    P.op("dve", lambda e: e.tensor_scalar(out=cv2[:, 0:1], in0=cv[:, CV_BDEC:CV_BDEC + 1], scalar1=-1.0, scalar2=None,
                                          op0=ALU.mult), reads=["cv"], writes=["cv2a"])
    P.op("dve", lambda e: e.tensor_scalar(out=cv2[:, 1:2], in0=cv[:, CV_SBQ:CV_SBQ + 1], scalar1=0.125, scalar2=None,
                                          op0=ALU.mult), reads=["cv"], writes=["cv2b"])
    if li == 0:
        P.op("dve", lambda e: e.memset(cv2[:, 2:4], 0.0), writes=["cv2c"])
        P.op("dve", lambda e: e.memset(cv2[:, 4:6], 1.0), writes=["cv2d"])
    else:
        P.op("dve", lambda e: e.tensor_tensor(out=cv2[:, 6:8], in0=cv[:, CV_LB:CV_LB + 2], in1=cv[:, CV_LB + 2:CV_LB + 4],
                                              op=ALU.subtract), reads=["cv"], writes=["cv2e"])
        P.op("act", lambda e: e.activation(out=cv2[:, 8:10], in_=cv2[:, 6:8], func=AF.Exp), reads=["cv2e"], writes=["cv2f"])
        P.op("act", lambda e: e.activation(out=cv2[:, 10:12], in_=cv2[:, 6:8], func=AF.Exp, scale=-1.0),
             reads=["cv2e"], writes=["cv2g"])
        P.op("dve", lambda e: e.tensor_scalar(out=cv2[:, 8:12], in0=cv2[:, 8:12], scalar1=1.0, scalar2=None, op0=ALU.add),
             reads=["cv2f", "cv2g"], writes=["cv2h"])
        P.op("dve", lambda e: e.reciprocal(out=cv2[:, 2:4], in_=cv2[:, 8:10]), reads=["cv2h"], writes=["cv2c"])
        P.op("dve", lambda e: e.reciprocal(out=cv2[:, 4:6], in_=cv2[:, 10:12]), reads=["cv2h"], writes=["cv2d"])
    P.op("dve", lambda e: e.memset(mo[0][:], 0.0), writes=[("mo", 0, j) for j in range(7)])
    P.op("dve", lambda e: e.memset(Sg[:], 0.0), writes=["Sg"])
    for h in range(2):
        P.op("dve", lambda e, h=h: e.memset(Sh[h][:], 0.0), writes=[("Sh", h)])

    rr = {"g": 0}

    def bank(kind):
        if kind == "w":
            return 3
        k = rr["g"] % 3
        rr["g"] += 1
        return k

    def headnorm(src, srckeys, M, gain, dst, dstkeys, extra=None, extrakeys=()):
        P.op("act", lambda e: e.activation(out=sqb[0:M, :], in_=src, func=AF.Square), reads=srckeys, writes=["sqb"])
        k = bank("g")
        P.op("pe", lambda e: e.matmul(pb[k][0:M, :], lhsT=ones(M), rhs=sqb[0:M, :], start=True, stop=True),
             reads=["sqb", "cb"], writes=[("pb", k)])
        P.op("act", lambda e: e.activation(out=lnv[0:M, :], in_=pb[k][0:M, :], func=AF.Ln, scale=1.0 / M, bias=EPS),
             reads=[("pb", k)], writes=["lnv"])
        P.op("act", lambda e: e.activation(out=rstd[0:M, :], in_=lnv[0:M, :], func=AF.Exp, scale=-0.5),
             reads=["lnv"], writes=["rstd"])
        if extra is None:
            P.op("dve", lambda e: e.scalar_tensor_tensor(out=dst, in0=src, scalar=gain, in1=rstd[0:M, :],
                                                         op0=ALU.mult, op1=ALU.mult),
                 reads=list(srckeys) + ["rstd", "cv", "cv2b"], writes=dstkeys)
        else:
            P.op("dve", lambda e: e.scalar_tensor_tensor(out=onrm[0:M, :], in0=src, scalar=gain, in1=rstd[0:M, :],
                                                         op0=ALU.mult, op1=ALU.mult),
                 reads=list(srckeys) + ["rstd", "cv"], writes=["onrm"])
            P.op("dve", lambda e: e.tensor_tensor(out=dst, in0=onrm[0:M, :], in1=extra, op=ALU.mult),
                 reads=["onrm"] + list(extrakeys), writes=dstkeys)

    def proj_fm(name):
        off, M = FM_OFF[name]
        k = bank("g")
        for c in range(8):
            P.op("pe", lambda e, c=c: e.matmul(pb[k][0:M, :], lhsT=win[:, c, off:off + M], rhs=hT[:, c, :],
                                               start=(c == 0), stop=(c == 7)),
                 reads=[("win", c), "hT"], writes=[("pb", k)])
        return k, M

    for ti in range(NTILE):
        tt = ti * 512
        mi = 0
        if "hT_in" in dr:
            P.dma("sp", "hTin", [(hT[:], dr["hT_in"][ti].rearrange("p (c t) -> p c t", c=8))], writes=["hT"])
        else:
            P.dma("sp", "xin", [(xt[:], dr["x"][tt:tt + 512, :].rearrange("(b p) d -> p b d", p=128))], writes=["xt"])
        for b in range(4 if "hT_in" not in dr else 0):
            P.op("act", lambda e, b=b: e.activation(out=sq[:], in_=xt[:, b, :], func=AF.Square, accum_out=stat[:, b:b + 1]),
                 reads=["xt"], writes=["sq", ("stat", b)])
            P.op("act", lambda e, b=b: e.activation(out=stat[:, 4 + b:5 + b], in_=stat[:, b:b + 1], func=AF.Ln,
                                                    scale=1.0 / D, bias=EPS), reads=[("stat", b)], writes=[("stat2", b)])
            P.op("act", lambda e, b=b: e.activation(out=stat[:, 8 + b:9 + b], in_=stat[:, 4 + b:5 + b], func=AF.Exp,
                                                    scale=-0.5), reads=[("stat2", b)], writes=[("stat3", b)])
            hi = b % 2
            P.op("dve", lambda e, b=b, hi=hi: e.scalar_tensor_tensor(out=hn[hi][:], in0=xt[:, b, :],
                                                                     scalar=stat[:, 8 + b:9 + b], in1=gbc[:],
                                                                     op0=ALU.mult, op1=ALU.mult),
                 reads=["xt", ("stat3", b), "gbc"], writes=[("hn", hi)])
            for c in range(8):
                P.op("pe", lambda e, c=c, hi=hi: e.transpose(ptr[:, c, :], hn[hi][:, c * 128:(c + 1) * 128], idn),
                     reads=[("hn", hi), "cb"], writes=["ptr"])
            P.op("act", lambda e, b=b: e.activation(out=hT[:, :, b * 128:(b + 1) * 128], in_=ptr[:], func=AF.Copy),
                 reads=["ptr"], writes=["hT"])
        if "hT_out" in dr:
            P.dma("sp", "hTout", [(dr["hT_out"][ti].rearrange("p (c t) -> p c t", c=8), hT[:])], reads=["hT"],
                  writes=[("hTs", ti)])
        for b in range(4):
            k = bank("g")
            for c in range(8):
                P.op("pe", lambda e, c=c, b=b, k=k: e.matmul(pb[k][:], lhsT=hT[:, c, b * 128:(b + 1) * 128],
                                                             rhs=win[:, c, TM_OFF:TM_OFF + 512], start=(c == 0), stop=(c == 7)),
                     reads=[("win", c), "hT"], writes=[("pb", k)])
            P.op("act", lambda e, b=b, k=k, ti=ti: e.activation(out=Vall[:, ti * 4 + b, :], in_=pb[k][:, 0:192], func=AF.Copy),
                 reads=[("pb", k)], writes=[("Vall", ti * 4 + b)])
            P.op("act", lambda e, b=b, k=k: e.activation(out=vt[:, b, :], in_=pb[k][:, 192:512], func=AF.Copy),
                 reads=[("pb", k)], writes=[("vt", b)])
        for h in range(2):
            k, M = proj_fm(f"gg{h}")
            P.op("act", lambda e, h=h, k=k: e.activation(out=sgl[h][:], in_=pb[k][0:96, :], func=AF.Silu),
                 reads=[("pb", k)], writes=[("sgl", h)])
        for h in range(2):
            k, M = proj_fm(f"hgt{h}")
            P.op("act", lambda e, h=h, k=k: e.activation(out=shg[h][:], in_=pb[k][0:64, :], func=AF.Silu),
                 reads=[("pb", k)], writes=[("shg", h)])

        sb_steps = []
        if "s" in parts:
            for h in range(3):
                k, M = proj_fm(f"sk{h}")
                ri = h % 2
                P.op("act", lambda e, k=k, ri=ri: e.activation(out=rawf[ri][0:64, :], in_=pb[k][0:64, :], func=AF.Copy),
                     reads=[("pb", k)], writes=[("rawf", ri)])
                headnorm(rawf[ri][0:64, :], [("rawf", ri)], 64, cv[0:64, CV_SBK:CV_SBK + 1],
                         KT[:, h, tt:tt + 512], [("KT", h, ti)])
                k, M = proj_fm(f"sq{h}")
                ri = (h + 1) % 2
                P.op("act", lambda e, k=k, ri=ri: e.activation(out=rawf[ri][0:64, :], in_=pb[k][0:64, :], func=AF.Copy),
                     reads=[("pb", k)], writes=[("rawf", ri)])
                headnorm(rawf[ri][0:64, :], [("rawf", ri)], 64, cv2[0:64, 1:2], qT[:, h, :], [("qT", h)])
            nkb = 4 * ti + 4
            pairs = [(h, idx, kb) for h in range(3) for idx, kb in enumerate(range(nkb - 1, -1, -1))]
            info = {}

            def stageA(n):
                h, idx, kb = pairs[n]
                r = kb - 4 * ti
                c0 = 128 * r if r >= 0 else 0
                ei = n % 2
                ktile = kb // 4
                kz = bank("z")
                info[n] = (c0, ei, r)
                P.op("pe", lambda e: e.matmul(pb[kz][:, c0:512], lhsT=KT[:, h, kb * 128:(kb + 1) * 128], rhs=qT[:, h, c0:512],
                                              start=True, stop=True),
                     reads=[("KT", h, ktile), ("qT", h)], writes=[("pb", kz)])
                P.op("act", lambda e: e.activation(out=ef[ei][:, c0:512], in_=pb[kz][:, c0:512], func=AF.Exp),
                     reads=[("pb", kz)], writes=[("ef", ei)])
                P.op("act", lambda e: e.activation(out=spb[ei][:, c0:512], in_=ef[ei][:, c0:512], func=AF.Ln, bias=1.0),
                     reads=[("ef", ei)], writes=[("spb", ei)])
                if r >= 0:
                    P.op("dve", lambda e: e.tensor_tensor(out=spb[ei][:, c0:c0 + 128], in0=spb[ei][:, c0:c0 + 128],
                                                          in1=cf[:, CF_STRICT:CF_STRICT + 128], op=ALU.mult),
                         reads=[("spb", ei), "cf"], writes=[("spb", ei)])

            def stageB1(n):
                h, idx, kb = pairs[n]
                c0, ei, r = info[n]
                first = (idx == 0)
                last = (kb == 0)
                ktile = kb // 4
                kw = bank("w")
                P.op("pe", lambda e: e.matmul(pb[kw][:, c0:512], lhsT=KT[:, h, kb * 128:(kb + 1) * 128], rhs=qT[:, h, c0:512],
                                              start=True, stop=False),
                     reads=[("KT", h, ktile), ("qT", h)], writes=[("pb", kw)])
                P.op("pe", lambda e: e.matmul(pb[kw][:, c0:512], lhsT=cb[:, CB_NEGINCL:CB_NEGINCL + 128], rhs=spb[ei][:, c0:512],
                                              start=False, stop=first),
                     reads=[("spb", ei), "cb"], writes=[("pb", kw)])
                if not first:
                    P.op("pe", lambda e: e.matmul(pb[kw][:, c0:512], lhsT=cb[:, CB_NEGONES:CB_NEGONES + 128], rhs=Sb[:, c0:512],
                                                  start=False, stop=True),
                         reads=["Sb", "cb"], writes=[("pb", kw)])
                P.op("act", lambda e: e.activation(out=wg[ei][:, c0:512], in_=pb[kw][:, c0:512], func=AF.Exp),
                     reads=[("pb", kw)], writes=[("wg", ei)])
                if r >= 0:
                    P.op("dve", lambda e: e.tensor_tensor(out=wg[ei][:, c0:c0 + 128], in0=wg[ei][:, c0:c0 + 128],
                                                          in1=cf[:, CF_STRICT:CF_STRICT + 128], op=ALU.mult),
                         reads=[("wg", ei), "cf"], writes=[("wg", ei)])
                if not last:
                    if first:
                        P.op("dve", lambda e: e.memset(Sb[:], 0.0), writes=["Sb"])
                    P.op("dve", lambda e: e.tensor_tensor(out=Sb[:, c0:512], in0=Sb[:, c0:512], in1=spb[ei][:, c0:512], op=ALU.add),
                         reads=[("spb", ei), "Sb"], writes=["Sb"])

            def stageB2(n):
                h, idx, kb = pairs[n]
                c0, ei, r = info[n]
                first = (idx == 0)
                last = (kb == 0)
                if first:
                    P.op("pe", lambda e: e.matmul(pb[6][0:64, :], lhsT=cb[:, CB_ZERO:CB_ZERO + 64], rhs=cb[:, 0:512],
                                                  start=True, stop=False), reads=["cb"], writes=[("pb", 6)])
                P.op("pe", lambda e: e.matmul(pb[6][0:64, c0:512], lhsT=Vall[:, kb, h * 64:(h + 1) * 64], rhs=wg[ei][:, c0:512],
                                              start=False, stop=last),
                     reads=[("wg", ei), ("Vall", kb)], writes=[("pb", 6)])
                if last:
                    headnorm(pb[6][0:64, :], [("pb", 6)], 64, cv[0:64, CV_SBO:CV_SBO + 1], mo[mi][0:64, 2 + h, :],
                             [("mo", mi, 2 + h)])

            npairs = len(pairs)
            sb_steps.append(lambda: stageA(0))
            for n in range(npairs):
                if n + 1 < npairs:
                    sb_steps.append(lambda n=n: stageA(n + 1))
                sb_steps.append(lambda n=n: stageB1(n))
                if n >= 1:
                    sb_steps.append(lambda n=n: stageB2(n - 1))
            sb_steps.append(lambda: stageB2(npairs - 1))

        def hg_gen():
            for h in range(2):
                k, M = proj_fm(f"hq{h}")
                P.op("act", lambda e, k=k, h=h: e.activation(out=qh[h][:], in_=pb[k][:], func=AF.Copy),
                     reads=[("pb", k)], writes=[("qh", h)])
                k, M = proj_fm(f"hf{h}")
                P.op("act", lambda e, k=k: e.activation(out=hfa[:], in_=pb[k][:], func=AF.Exp, scale=-1.0),
                     reads=[("pb", k)], writes=["hfa"])
                yield
                P.op("dve", lambda e: e.tensor_scalar(out=hfa[:], in0=hfa[:], scalar1=1.0, scalar2=None, op0=ALU.add),
                     reads=["hfa"], writes=["hfa"])
                P.op("dve", lambda e: e.reciprocal(out=hfb[:], in_=hfa[:]), reads=["hfa"], writes=["hfb"])
                P.op("act", lambda e, h=h: e.activation(out=hfb[:], in_=hfb[:], func=AF.Identity, scale=cv2[:, 4 + h:5 + h],
                                                        bias=cv2[:, 2 + h:3 + h]),
                     reads=["hfb", "cv2c", "cv2d"], writes=["hfb"])
                P.op("act", lambda e: e.activation(out=lf[:], in_=hfb[:], func=AF.Ln), reads=["hfb"], writes=["lf"])
                P.op("act", lambda e, h=h: e.activation(out=kh[h][:], in_=hfb[:], func=AF.Identity, scale=-1.0, bias=1.0),
                     reads=["hfb"], writes=[("kh", h)])
                yield
                yield from lin_attn(P, L_, "hg", ti, mi, h)

        def gla_gen():
            k, M = proj_fm("gq")
            P.op("act", lambda e, k=k: e.activation(out=qg[:], in_=pb[k][:], func=AF.Copy, scale=48.0 ** -0.5),
                 reads=[("pb", k)], writes=["qg"])
            k, M = proj_fm("gk")
            P.op("act", lambda e, k=k: e.activation(out=kg[:], in_=pb[k][:], func=AF.Copy), reads=[("pb", k)], writes=["kg"])
            yield
            k, M = proj_fm("glr")
            P.op("act", lambda e, k=k: e.activation(out=glr[:], in_=pb[k][0:16, :], func=AF.Copy), reads=[("pb", k)], writes=["glr"])
            k = bank("g")
            P.op("pe", lambda e, k=k: e.matmul(pb[k][:], lhsT=wdec[:], rhs=glr[:], start=True, stop=True),
                 reads=["wdec", "glr"], writes=[("pb", k)])
            P.op("act", lambda e, k=k: e.activation(out=hfa[:], in_=pb[k][:], func=AF.Exp, scale=-1.0, bias=cv2[:, 0:1]),
                 reads=[("pb", k), "cv2a"], writes=["hfa"])
            P.op("act", lambda e: e.activation(out=lf[:], in_=hfa[:], func=AF.Ln, bias=1.0), reads=["hfa"], writes=["lf"])
            yield
            yield from lin_attn(P, L_, "gla", ti, mi)

        def bg_gen():
            if "h" in parts:
                yield from hg_gen()
            if "g" in parts:
                yield from gla_gen()

        L_ = dict(locals())
        bg = bg_gen()
        n_bg = 2 * 11 + 8
        every = max(1, len(sb_steps) // n_bg) if sb_steps else 1
        for i, st in enumerate(sb_steps):
            st()
            if i % every == every - 1:
                next(bg, None)
        for _ in bg:
            pass

        outs = []
        rows = [(0, 96), (96, 96), (192, 64), (256, 64), (320, 64), (384, 64), (448, 64)]
        for j, (r0, m) in enumerate(rows):
            outs.append((dr["mT"][r0:r0 + m, tt:tt + 512], mo[mi][0:m, j, :]))
        P.dma("sp", f"mout{mi}", outs, reads=[("mo", mi, j) for j in range(7)], writes=[("mT", ti)])


def lin_attn(P, L, kind, ti, mi, h=0):
    cf, cb, pb, ptr = L["cf"], L["cb"], L["pb"], L["ptr"]
    Gs, dlt, E1, E2, dl, qd, kd = L["Gs"], L["dlt"], L["E1"], L["E2"], L["dl"], L["qd"], L["kd"]
    ATs, ktok, vt, lf = L["ATs"], L["ktok"], L["vt"], L["lf"]
    bank, headnorm, mo, cv = L["bank"], L["headnorm"], L["mo"], L["cv"]
    idn = L["idn"]
    if kind == "gla":
        C, nb, s1, s2 = 128, 4, 1.0 / 16, -1.0 / 16
        q, k, qkeys, kkeys = L["qg"], L["kg"], ["qg"], ["kg"]
        roff, mask = CF_R128, CF_TRI
    else:
        C, nb, s1, s2 = 64, 8, -1.0, 1.0
        q, k, qkeys, kkeys = L["qh"][h], L["kh"][h], [("qh", h)], [("kh", h)]
        roff, mask = CF_R64, CF_BD64
    P.op("dve", lambda e: e.tensor_tensor_scan(out=Gs[:], data0=cf[:, roff:roff + 512], data1=lf[:], initial=0.0,
                                               op0=ALU.mult, op1=ALU.add), reads=["lf", "cf"], writes=["Gs"])
    gv = Gs[:].rearrange("p (b c) -> p b c", c=C)
    P.op("dve", lambda e: e.tensor_tensor(out=dlt[:].rearrange("p (b c) -> p b c", c=C),
                                          in0=gv[:, :, C - 1:C].to_broadcast([128, nb, C]), in1=gv, op=ALU.subtract),
         reads=["Gs"], writes=["dlt"])
    P.op("act", lambda e: e.activation(out=E1[:], in_=dlt[:], func=AF.Exp, scale=s1), reads=["dlt"], writes=["E1"])
    P.op("act", lambda e: e.activation(out=E2[:], in_=dlt[:], func=AF.Exp, scale=-s1), reads=["dlt"], writes=["E2"])
    P.op("act", lambda e: e.activation(out=dl[:, 0:nb], in_=gv[:, :, C - 1], func=AF.Exp, scale=s2), reads=["Gs"], writes=["dl"])
    P.op("dve", lambda e: e.tensor_tensor(out=qd[:], in0=q[:], in1=E1[:], op=ALU.mult), reads=qkeys + ["E1"], writes=["qd"])
    P.op("dve", lambda e: e.tensor_tensor(out=kd[:], in0=k[:], in1=E2[:], op=ALU.mult), reads=kkeys + ["E2"], writes=["kd"])
    if kind != "gla":
        for s in range(2):
            P.op("dve", lambda e, s=s: e.tensor_tensor(out=L["qz"][s][:], in0=qd[:], in1=cf[:, CF_M0 + 512 * s:CF_M0 + 512 * (s + 1)], op=ALU.mult),
                 reads=["qd", "cf"], writes=[("qz", s)])
    yield
    for b4 in range(4):
        cs = slice(b4 * 128, (b4 + 1) * 128)
        ai = b4 % 2
        P.op("pe", lambda e, cs=cs: e.transpose(ptr[:, 0, :], kd[:, cs], idn), reads=["kd", "cb"], writes=["ptr"])
        P.op("act", lambda e, ai=ai: e.activation(out=ktok[ai][:], in_=ptr[:, 0, :], func=AF.Copy), reads=["ptr"], writes=[("ktok", ai)])
        if kind == "gla":
            Sg, Sgb = L["Sg"], L["Sgb"]
            P.op("dve", lambda e, b4=b4: e.tensor_scalar(out=Sgb[:], in0=Sg[:], scalar1=dl[:, b4:b4 + 1], scalar2=None, op0=ALU.mult),
                 reads=["Sg", "dl"], writes=["Sgb"])
            ku = bank("w")
            for hh in range(2):
                ps = slice(64 * hh, 64 * hh + 64)
                ka = bank("g")
                P.op("pe", lambda e, ka=ka, ps=ps, cs=cs: e.matmul(pb[ka][:, 0:128], lhsT=kd[ps, cs], rhs=qd[ps, cs], start=True, stop=True),
                     reads=["kd", "qd"], writes=[("pb", ka)])
                P.op("dve", lambda e, ka=ka, hh=hh: e.tensor_tensor(out=ATs[hh][:], in0=pb[ka][:, 0:128], in1=cf[:, mask:mask + 128], op=ALU.mult),
                     reads=[("pb", ka), "cf"], writes=[("ATs", hh)])
                acc = 6
                P.op("pe", lambda e, hh=hh, b4=b4, cs=cs: e.matmul(pb[4 + hh][0:96, cs],
                                                                   lhsT=vt[:, b4, hh * 96:(hh + 1) * 96], rhs=ATs[hh][:], start=True, stop=False),
                     reads=[("ATs", hh), ("vt", b4)], writes=[("pb", 4 + hh)])
                P.op("pe", lambda e, hh=hh, ps=ps, cs=cs: e.matmul(pb[4 + hh][0:96, cs],
                                                                   lhsT=Sgb[ps, :], rhs=qd[ps, cs], start=False, stop=True),
                     reads=["Sgb", "qd"], writes=[("pb", 4 + hh)])
                P.op("pe", lambda e, hh=hh, ps=ps, b4=b4, ai=ai, ku=ku: e.matmul(pb[ku][ps, 0:96], lhsT=ktok[ai][:, ps],
                                                                                rhs=vt[:, b4, hh * 96:(hh + 1) * 96], start=True, stop=True),
                     reads=[("ktok", ai), ("vt", b4)], writes=[("pb", ku)])
            P.op("dve", lambda e, b4=b4, ku=ku: e.scalar_tensor_tensor(out=Sg[:], in0=Sg[:], scalar=dl[:, b4:b4 + 1], in1=pb[ku][:, 0:96],
                                                                      op0=ALU.mult, op1=ALU.add),
                 reads=["Sg", "dl", ("pb", ku)], writes=["Sg"])
            yield
        else:
            Sh, Shb = L["Sh"][h], L["Shb"][h]
            ka = bank("g")
            P.op("pe", lambda e, ka=ka, cs=cs: e.matmul(pb[ka][:, 0:128], lhsT=kd[:, cs], rhs=qd[:, cs], start=True, stop=True),
                 reads=["kd", "qd"], writes=[("pb", ka)])
            P.op("dve", lambda e, ka=ka: e.tensor_tensor(out=ATs[0][:], in0=pb[ka][:, 0:128], in1=cf[:, mask:mask + 128], op=ALU.mult),
                 reads=[("pb", ka), "cf"], writes=[("ATs", 0)])
            P.op("pe", lambda e, b4=b4, cs=cs: e.matmul(pb[4][0:64, cs], lhsT=vt[:, b4, 192 + h * 64:192 + (h + 1) * 64], rhs=ATs[0][:],
                                                        start=True, stop=False),
                 reads=[("ATs", 0), ("vt", b4)], writes=[("pb", 4)])
            for s in range(2):
                blk = 2 * b4 + s
                P.op("dve", lambda e, s=s, b4=b4: e.tensor_scalar(out=L["vz"][s][:], in0=vt[:, b4, 192 + h * 64:192 + (h + 1) * 64],
                                                               scalar1=cf[:, CF_P0 + s:CF_P0 + s + 1], scalar2=None, op0=ALU.mult),
                     reads=[("vt", b4), "cf"], writes=[("vz", s)])
                P.op("dve", lambda e, blk=blk: e.tensor_scalar(out=Shb[:], in0=Sh[:], scalar1=dl[:, blk:blk + 1], scalar2=None, op0=ALU.mult),
                     reads=[("Sh", h), "dl"], writes=[("Shb", h)])
                P.op("pe", lambda e, cs=cs, s=s: e.matmul(pb[4][0:64, cs], lhsT=Shb[:], rhs=L["qz"][s][:, cs], start=False, stop=(s == 1)),
                     reads=[("Shb", h), ("qz", s)], writes=[("pb", 4)])
                ku = bank("g")
                P.op("pe", lambda e, ku=ku, s=s, ai=ai: e.matmul(pb[ku][:, 0:64], lhsT=ktok[ai][:], rhs=L["vz"][s][:], start=True, stop=True),
                     reads=[("ktok", ai), ("vz", s)], writes=[("pb", ku)])
                P.op("dve", lambda e, blk=blk, ku=ku: e.scalar_tensor_tensor(out=Sh[:], in0=Sh[:], scalar=dl[:, blk:blk + 1], in1=pb[ku][:, 0:64],
                                                                            op0=ALU.mult, op1=ALU.add),
                     reads=[("Sh", h), "dl", ("pb", ku)], writes=[("Sh", h)])
                yield
    if kind == "gla":
        for hh in range(2):
            src = pb[4 + hh][0:96, :]
            headnorm(src, [("pb", 4 + hh)], 96, cv[0:96, CV_GLAG:CV_GLAG + 1], mo[mi][0:96, hh, :], [("mo", mi, hh)],
                     extra=L["sgl"][hh][:], extrakeys=[("sgl", hh)])
    else:
        headnorm(pb[4][0:64, :], [("pb", 4)], 64, cv[0:64, CV_HGO:CV_HGO + 1], mo[mi][0:64, 5 + h, :], [("mo", mi, 5 + h)],
                 extra=L["shg"][h][:], extrakeys=[("shg", h)])


def mixer_rows(hh):
    rows = []
    for j in range(2):
        rows.append((96 * j, 96, (2 * hh + j) * 96))
    for j in range(3):
        rows.append((192 + 64 * j, 64, 384 + (3 * hh + j) * 64))
    for j in range(2):
        rows.append((384 + 64 * j, 64, 768 + (2 * hh + j) * 64))
    return rows


_CONSTS = None


def mixer_inputs(p, li, hh, xb):
    global _CONSTS
    if _CONSTS is None:
        _CONSTS = make_consts()
    W = p["w_in"][li]
    offs = np.concatenate([[0], np.cumsum([192, 192, 384, 16, 384, 384, 384, 384, 512, 512, 256, 256])])
    (o_gq, o_gk, o_gv, o_lr, o_gg, o_sq, o_sk, o_sv, o_hq, o_hf, o_hi, o_hg) = offs[:12]
    wc = np.zeros((D, WC), np.float32)

    def put(name, src0, n, dst_off=0):
        off, M = FM_OFF[name]
        wc[:, off + dst_off:off + dst_off + n] = W[:, src0:src0 + n]

    for j in range(2):
        hd = 2 * hh + j
        put("gq", o_gq + hd * 48, 48, 64 * j)
        put("gk", o_gk + hd * 48, 48, 64 * j)
        put(f"gg{j}", o_gg + hd * 96, 96)
        put(f"hq{j}", o_hq + hd * 128, 128)
        put(f"hf{j}", o_hf + hd * 128, 128)
        put(f"hgt{j}", o_hg + hd * 64, 64)
        wc[:, TM_OFF + 192 + 96 * j:TM_OFF + 192 + 96 * (j + 1)] = W[:, o_gv + hd * 96:o_gv + (hd + 1) * 96]
        wc[:, TM_OFF + 384 + 64 * j:TM_OFF + 384 + 64 * (j + 1)] = W[:, o_hi + hd * 64:o_hi + (hd + 1) * 64]
    put("glr", o_lr, 16)
    for j in range(3):
        hd = 3 * hh + j
        put(f"sq{j}", o_sq + hd * 64, 64)
        put(f"sk{j}", o_sk + hd * 64, 64)
        wc[:, TM_OFF + 64 * j:TM_OFF + 64 * (j + 1)] = W[:, o_sv + hd * 64:o_sv + (hd + 1) * 64]
    cvec = np.zeros((128, 16), np.float32)
    wdec = np.zeros((16, 128), np.float32)
    for j in range(2):
        hd = 2 * hh + j
        cvec[64 * j:64 * j + 48, CV_BDEC] = p["gla_b_decay"][li][hd * 48:(hd + 1) * 48]
        wdec[:, 64 * j:64 * j + 48] = p["gla_w_decay"][li][:, hd * 48:(hd + 1) * 48]
        cvec[:, CV_LB + j] = p["hg_lb_logits"][0][hd * 128:(hd + 1) * 128]
        cvec[:, CV_LB + 2 + j] = p["hg_lb_logits"][min(1, p["hg_lb_logits"].shape[0] - 1)][hd * 128:(hd + 1) * 128]
    cvec[0:96, CV_GLAG] = p["gla_out_g"][li]
    cvec[0:64, CV_SBQ] = p["sb_q_g"][li]
    cvec[0:64, CV_SBK] = p["sb_k_g"][li]
    cvec[0:64, CV_SBO] = p["sb_out_g"][li]
    cvec[0:64, CV_HGO] = p["hg_out_g"][li]
    return dict(x=np.ascontiguousarray(xb), w_in=wc,
                g_bc=np.ascontiguousarray(np.broadcast_to(p["norm_mix_g"][li], (128, D))),
                cvec=cvec, wdec=wdec, cf=_CONSTS[0], cb=_CONSTS[1])


_NC_CACHE = {}


def _get(kind, *args):
    key = (kind,) + args
    if key not in _NC_CACHE:
        _NC_CACHE[key] = build_mixer(*args) if kind == "mix" else build_ffn(*args)
    return _NC_CACHE[key]


def kernel_unfused(**inp):
    p = {k: np.asarray(v) for k, v in inp.items()}
    x = np.ascontiguousarray(p["x"], dtype=np.float32)
    B, T, _ = x.shape
    depth = p["w_in"].shape[0]
    ident = np.eye(128, dtype=np.float32).astype(NPBF)
    cores = list(range(8))
    for li in range(depth):
        nc = _get("mix", T, li, "sgh")
        in_maps = [mixer_inputs(p, li, c % 2, x[c // 2]) for c in cores]
        res = run_bass_kernel_spmd(nc, in_maps, core_ids=cores).results
        mT = np.zeros((B, D, T), NPBF)
        for c in cores:
            b, hh = c // 2, c % 2
            m = res[c]["mT"]
            for (r0, n, g0) in mixer_rows(hh):
                mT[b, g0:g0 + n, :] = m[r0:r0 + n, :]
        nc2 = _get("ffn", T // 2)
        in_maps = []
        for c in cores:
            b, th = c // 2, c % 2
            sl = slice(th * (T // 2), (th + 1) * (T // 2))
            in_maps.append(dict(x=np.ascontiguousarray(x[b, sl]), mT=np.ascontiguousarray(mT[b][:, sl]),
                                w_out=p["w_out"][li], w_up=p["w_ffn_up"][li], w_down=p["w_ffn_down"][li],
                                g_bc=np.ascontiguousarray(np.broadcast_to(p["norm_ffn_g"][li], (128, D))), ident=ident))
        res = run_bass_kernel_spmd(nc2, in_maps, core_ids=cores).results
        xn = np.empty_like(x)
        for c in cores:
            b, th = c // 2, c % 2
            xn[b, th * (T // 2):(th + 1) * (T // 2)] = res[c]["y"]
        x = xn
    return x


def build_fused(T, depth=2):
    nc = bass.Bass("TRN2", target_bir_lowering=False)
    x = nc.dram_tensor("x", [T, D], F32, kind="ExternalInput").ap()
    w_in = nc.dram_tensor("w_in", [depth, 2, D, WC], F32, kind="ExternalInput").ap()
    gmix = nc.dram_tensor("gmix", [depth, 128, D], F32, kind="ExternalInput").ap()
    gffn = nc.dram_tensor("gffn", [depth, 128, D], F32, kind="ExternalInput").ap()
    cvec = nc.dram_tensor("cvec", [depth, 2, 128, 16], F32, kind="ExternalInput").ap()
    wdec = nc.dram_tensor("wdec", [depth, 2, 16, 128], F32, kind="ExternalInput").ap()
    cf = nc.dram_tensor("cf", [128, CF_W], F32, kind="ExternalInput").ap()
    cb = nc.dram_tensor("cb", [128, CB_W], BF16, kind="ExternalInput").ap()
    w_out = nc.dram_tensor("w_out", [depth, D, D], F32, kind="ExternalInput").ap()
    w_up = nc.dram_tensor("w_up", [depth, D, 2 * DFF], F32, kind="ExternalInput").ap()
    w_down = nc.dram_tensor("w_down", [depth, DFF, D], F32, kind="ExternalInput").ap()
    y = nc.dram_tensor("y", [T, D], F32, kind="ExternalOutput").ap()
    mTs = nc.dram_tensor("mTs", [D, T], BF16).ap()
    hTs = nc.dram_tensor("hTs", [T // 512, 128, 8 * 512], BF16).ap()
    xs = nc.dram_tensor("xs", [T, D], F32).ap()
    unit = min(UNIT, T)
    for li in range(depth):
        xin = x if li == 0 else xs
        for hh in range(2):
            A = Alloc(nc)
            P = Prog(nc)
            dr = dict(x=xin, w_in=w_in[li, hh], g_bc=gmix[li], cvec=cvec[li, hh], wdec=wdec[li, hh], cf=cf, cb=cb,
                      mT=mTs[hh * 512:(hh + 1) * 512, :])
            dr["hT_out" if hh == 0 else "hT_in"] = hTs
            emit_mixer(nc, A, P, dr, T, li, "sgh")
            P.wait_all("sp", [("mT", t) for t in range(T // 512)] + ([("hTs", t) for t in range(T // 512)] if hh == 0 else []))
            P.emit()
            A.close()
        A = Alloc(nc)
        P = Prog(nc)
        emit_ffn(nc, A, P, xin, mTs, w_out[li], w_up[li], w_down[li], gffn[li], cb[:, CB_IDENT:CB_IDENT + 128],
                 (y if li == depth - 1 else xs), T, unit, T // unit)
        P.wait_all("sp", [("y", u) for u in range(T // unit)])
        P.emit()
        A.close()
    return nc


def fused_inputs(p, b):
    global _CONSTS
    if _CONSTS is None:
        _CONSTS = make_consts()
    depth = p["w_in"].shape[0]
    w_in = np.zeros((depth, 2, D, WC), np.float32)
    cvec = np.zeros((depth, 2, 128, 16), np.float32)
    wdec = np.zeros((depth, 2, 16, 128), np.float32)
    w_out = np.zeros((depth, D, D), np.float32)
    for li in range(depth):
        for hh in range(2):
            m = mixer_inputs(p, li, hh, p["x"][b])
            w_in[li, hh] = m["w_in"]
            cvec[li, hh] = m["cvec"]
            wdec[li, hh] = m["wdec"]
            for (r0, n, g0) in mixer_rows(hh):
                w_out[li, hh * 512 + r0:hh * 512 + r0 + n, :] = p["w_out"][li][g0:g0 + n, :]
    return dict(x=np.ascontiguousarray(p["x"][b]), w_in=w_in,
                gmix=np.ascontiguousarray(np.broadcast_to(p["norm_mix_g"][:, None, :], (depth, 128, D))),
                gffn=np.ascontiguousarray(np.broadcast_to(p["norm_ffn_g"][:, None, :], (depth, 128, D))),
                cvec=cvec, wdec=wdec, cf=_CONSTS[0], cb=_CONSTS[1], w_out=w_out,
                w_up=np.ascontiguousarray(p["w_ffn_up"]), w_down=np.ascontiguousarray(p["w_ffn_down"]))


def kernel(**inp):
    p = {k: np.asarray(v) for k, v in inp.items()}
    p["x"] = np.ascontiguousarray(p["x"], dtype=np.float32)
    B, T, _ = p["x"].shape
    nc = _get_fused(T, p["w_in"].shape[0])
    cores = list(range(8))
    per_b = [fused_inputs(p, b) for b in range(B)]
    in_maps = [per_b[c % B] for c in cores]
    res = run_bass_kernel_spmd(nc, in_maps, core_ids=cores).results
    return np.stack([res[b]["y"] for b in range(B)], axis=0)


def _get_fused(T, depth):
    key = ("fused", T, depth)
    if key not in _NC_CACHE:
        _NC_CACHE[key] = build_fused(T, depth)
    return _NC_CACHE[key]
```

```python
import numpy as np
import ml_dtypes
import concourse.bass as bass
import concourse.mybir as mybir
from concourse.bass_utils import run_bass_kernel_spmd

F32 = mybir.dt.float32
BF16 = mybir.dt.bfloat16
AF = mybir.ActivationFunctionType
ALU = mybir.AluOpType
AX = mybir.AxisListType
NPBF = ml_dtypes.bfloat16

D = 1024
DFF = 2816
EPS = 1e-6


class Prog:
    ENGS = ("pe", "act", "dve", "pool", "sp")

    PHASE = [0]

    def __init__(self, nc, same_eng_sync=True):
        self.nc = nc
        Prog.PHASE[0] += 1
        self.pfx = f"p{Prog.PHASE[0]}_"
        self.q = {e: [] for e in self.ENGS}
        self.cnt = {}
        self.last_w = {}
        self.readers = {}
        self.waited = {e: {} for e in self.ENGS}
        self.same = same_eng_sync
        self.sem_names = set(self.ENGS)
        self.n_ops = 0

    def _deps(self, eng, reads, writes, own_sem):
        need = {}

        def add(sv):
            if sv is None:
                return
            s, v = sv
            if s == own_sem and (not self.same or eng == "pe"):
                return
            if need.get(s, 0) < v:
                need[s] = v

        for k in reads:
            add(self.last_w.get(k))
        for k in writes:
            add(self.last_w.get(k))
            for s, v in self.readers.get(k, {}).items():
                add((s, v))
        out = []
        for s, v in need.items():
            if self.waited[eng].get(s, 0) < v:
                self.waited[eng][s] = v
                out.append((s, v))
        return out

    def _mark(self, reads, writes, sem, val):
        for k in reads:
            self.readers.setdefault(k, {})[sem] = val
        for k in writes:
            self.last_w[k] = (sem, val)
            self.readers[k] = {}

    def op(self, eng, fn, reads=(), writes=()):
        import os
        if self.n_ops >= int(os.environ.get("PROG_MAXOPS", "100000000")):
            return
        waits = self._deps(eng, reads, writes, eng)
        v = self.cnt.get(eng, 0) + 1
        self.cnt[eng] = v
        self.q[eng].append((waits, fn, eng, 1))
        self._mark(reads, writes, eng, v)
        self.n_ops += 1

    def dma(self, queue, sem, pairs, reads=(), writes=(), **kw):
        import os
        if self.n_ops >= int(os.environ.get("PROG_MAXOPS", "100000000")):
            return
        self.n_ops += 1
        self.sem_names.add(sem)
        waits = self._deps(queue, reads, writes, None)
        base = self.cnt.get(sem, 0)
        for i, (o, i_) in enumerate(pairs):
            def fn(e, o=o, i_=i_):
                return e.dma_start(out=o, in_=i_, **kw)
            self.q[queue].append((waits if i == 0 else [], fn, sem, 16))
        v = base + 16 * len(pairs)
        self.cnt[sem] = v
        self._mark(reads, writes, sem, v)

    def wait_all(self, eng, keys):
        waits = self._deps(eng, keys, (), None)
        import os
        if os.environ.get("PROG_MAXOPS"):
            waits = [(s_, v) for s_, v in self.cnt.items() if v > 0]
        self.q[eng].append((waits, None, None, 0))

    def emit(self):
        nc = self.nc
        import contextlib
        with contextlib.ExitStack() as st:
            sems = {n: st.enter_context(nc.semaphore(self.pfx + "s_" + n)) for n in sorted(self.sem_names)}
            block = st.enter_context(nc.Block())

            def replay(name):
                def run(e):
                    for waits, fn, sem, inc in self.q[name]:
                        for s, v in waits:
                            e.wait_ge(sems[s], v)
                        if fn is not None:
                            ins = fn(e)
                            ins.then_inc(sems[sem], inc)
                return run

            block.sync(replay("sp"))
            block.tensor(replay("pe"))
            block.scalar(replay("act"))
            block.vector(replay("dve"))
            block.gpsimd(replay("pool"))


class Alloc:
    CNT = [0]

    def __init__(self, nc):
        import contextlib
        self.nc = nc
        self.st = contextlib.ExitStack()
        Alloc.CNT[0] += 1
        self.pfx = f"a{Alloc.CNT[0]}_"

    def sb(self, name, shape, dt):
        return self.st.enter_context(self.nc.sbuf_tensor(self.pfx + "sb_" + name, list(shape), dt))

    def ps(self, name, shape, dt):
        return self.st.enter_context(self.nc.psum_tensor(self.pfx + "ps_" + name, list(shape), dt))

    def close(self):
        self.st.close()


FF_GROUPS = [(0, 4), (4, 4), (8, 4), (12, 4), (16, 3), (19, 3)]
UNIT = 1024


def build_ffn(NT):
    assert NT % UNIT == 0 or NT == 512
    unit = min(UNIT, NT)
    n_units = NT // unit
    nc = bass.Bass("TRN2", target_bir_lowering=False)
    x = nc.dram_tensor("x", [NT, D], F32, kind="ExternalInput").ap()
    mT = nc.dram_tensor("mT", [D, NT], BF16, kind="ExternalInput").ap()
    w_out = nc.dram_tensor("w_out", [D, D], F32, kind="ExternalInput").ap()
    w_up = nc.dram_tensor("w_up", [D, 2 * DFF], F32, kind="ExternalInput").ap()
    w_down = nc.dram_tensor("w_down", [DFF, D], F32, kind="ExternalInput").ap()
    g_bc = nc.dram_tensor("g_bc", [128, D], F32, kind="ExternalInput").ap()
    ident = nc.dram_tensor("ident", [128, 128], BF16, kind="ExternalInput").ap()
    y = nc.dram_tensor("y", [NT, D], F32, kind="ExternalOutput").ap()

    A = Alloc(nc)
    P = Prog(nc)
    emit_ffn(nc, A, P, x, mT, w_out, w_up, w_down, g_bc, ident, y, NT, unit, n_units)
    P.wait_all("sp", [("y", u) for u in range(n_units)])
    P.emit()
    A.close()
    return nc


def emit_ffn(nc, A, P, x, mT, w_out, w_up, w_down, g_bc, ident, y, NT, unit, n_units):
    ntile = unit // 512
    nblk = unit // 128
    wout_sb = A.sb("wout_sb", [128, 8, D], BF16)
    stage = [A.sb(f"stage{i}", [128, 2048], F32) for i in range(2)]
    xres = A.sb("xres", [128, nblk, D], F32)
    hnT = A.sb("hnT", [128, 8, unit], BF16)
    mts = [A.sb(f"mts{i}", [128, 8, 512], BF16) for i in range(2)]
    wup = [A.sb(f"wup{i}", [128, 8, 2, 512], BF16) for i in range(2)]
    wdn = [A.sb(f"wdn{i}", [128, 4, D], BF16) for i in range(2)]
    actT = [A.sb(f"actT{i}", [128, 4, 512], BF16) for i in range(2)]
    sg = [A.sb(f"sg{i}", [128, 512], F32) for i in range(2)]
    gbc = A.sb("gbc", [128, D], F32)
    idn = A.sb("idn", [128, 128], BF16)
    hn = [A.sb(f"hn{i}", [128, D], BF16) for i in range(2)]
    sq = A.sb("sq", [128, D], BF16)
    stat = A.sb("stat", [128, 64], F32)
    pb = [A.ps(f"pb{i}", [128, 512], F32) for i in range(7)]
    ptr = A.ps("ptr", [128, 8, 128], BF16)

    st_i = [0]

    def load_cast(dst_ap, src_ap, shape, dkey):
        i = st_i[0] % 2
        st_i[0] += 1
        n = int(np.prod(shape))
        sv = stage[i][:, 0:n]
        if len(shape) == 2:
            sv = sv.rearrange("p (a b) -> p a b", a=shape[0])
        P.dma("sp", f"stg{i}", [(sv, src_ap)], writes=[("stage", i)])
        if i == 0:
            P.op("pool", lambda e: e.tensor_copy(out=dst_ap, in_=sv),
                 reads=[("stage", i)], writes=[dkey])
        else:
            P.op("act", lambda e: e.activation(out=dst_ap, in_=sv, func=AF.Copy),
                 reads=[("stage", i)], writes=[dkey])

    P.dma("sp", "const", [(gbc[:], g_bc), (idn[:], ident)], writes=["gbc", "idn"])
    for c2 in range(4):
        load_cast(wout_sb[:, 2 * c2:2 * c2 + 2, :],
                  w_out[c2 * 256:(c2 + 1) * 256, :].rearrange("(c p) n -> p c n", p=128),
                  (2, D), ("wout", c2))

    pbi = [0]

    def next_pb(lo, hi):
        i = lo + pbi[0] % (hi - lo)
        pbi[0] += 1
        return i

    for u in range(n_units):
        t0 = u * unit
        for t in range(ntile):
            tt = t0 + t * 512
            mi = (u * ntile + t) % 2
            P.dma("sp", f"mts{mi}", [(mts[mi][:], mT[:, tt:tt + 512].rearrange("(c p) t -> p c t", p=128))],
                  writes=[("mts", mi)])
            xv = xres[:, 4 * t:4 * t + 4, :]
            P.dma("sp", f"xin{t % 2}", [(xv, x[tt:tt + 512, :].rearrange("(b p) d -> p b d", p=128))],
                  writes=[("xres", 4 * t + b) for b in range(4)])
            for b in range(4):
                bb = 4 * t + b
                for h in range(2):
                    k = next_pb(0, 4)
                    for c in range(8):
                        P.op("pe", lambda e, k=k, c=c, b=b, h=h, mi=mi: e.matmul(
                            pb[k][:], lhsT=mts[mi][:, c, b * 128:(b + 1) * 128],
                            rhs=wout_sb[:, c, h * 512:(h + 1) * 512], start=(c == 0), stop=(c == 7)),
                            reads=[("mts", mi), ("wout", c // 2)], writes=[("pb", k)])
                    P.op("dve", lambda e, k=k, bb=bb, h=h: e.tensor_tensor(
                        out=xres[:, bb, h * 512:(h + 1) * 512], in0=pb[k][:],
                        in1=xres[:, bb, h * 512:(h + 1) * 512], op=ALU.add),
                        reads=[("pb", k)], writes=[("xres", bb)])
                P.op("act", lambda e, bb=bb: e.activation(out=sq[:], in_=xres[:, bb, :], func=AF.Square,
                                                          accum_out=stat[:, bb:bb + 1]),
                     reads=[("xres", bb)], writes=["sq", ("stat", bb)])
                P.op("act", lambda e, bb=bb: e.activation(out=stat[:, 16 + bb:17 + bb], in_=stat[:, bb:bb + 1],
                                                          func=AF.Ln, scale=1.0 / D, bias=EPS),
                     reads=[("stat", bb)], writes=[("stat2", bb)])
                P.op("act", lambda e, bb=bb: e.activation(out=stat[:, 32 + bb:33 + bb], in_=stat[:, 16 + bb:17 + bb],
                                                          func=AF.Exp, scale=-0.5),
                     reads=[("stat2", bb)], writes=[("stat3", bb)])
                hi = bb % 2
                P.op("dve", lambda e, bb=bb, hi=hi: e.scalar_tensor_tensor(
                    out=hn[hi][:], in0=xres[:, bb, :], scalar=stat[:, 32 + bb:33 + bb], in1=gbc[:],
                    op0=ALU.mult, op1=ALU.mult),
                    reads=[("xres", bb), ("stat3", bb), "gbc"], writes=[("hn", hi)])
                for c in range(8):
                    P.op("pe", lambda e, c=c, hi=hi: e.transpose(ptr[:, c, :], hn[hi][:, c * 128:(c + 1) * 128], idn[:]),
                         reads=[("hn", hi), "idn"], writes=["ptr"])
                P.op("act", lambda e, bb=bb: e.activation(out=hnT[:, :, bb * 128:(bb + 1) * 128], in_=ptr[:],
                                                          func=AF.Copy),
                     reads=["ptr"], writes=[("hnT", bb)])
        for gi, (j0, G) in enumerate(FF_GROUPS):
            wi = (u * len(FF_GROUPS) + gi) % 2
            gw = G * 128
            for s in range(2):
                for ch in range(2):
                    c0 = s * DFF + j0 * 128
                    load_cast(wup[wi][:, 4 * ch:4 * ch + 4, s, 0:gw],
                              w_up[ch * 512:(ch + 1) * 512, c0:c0 + gw].rearrange("(c p) n -> p c n", p=128),
                              (4, gw), ("wup", wi, s, ch))
            for jh in range(0, G, 2):
                n = min(2, G - jh)
                load_cast(wdn[wi][:, jh:jh + n, :],
                          w_down[(j0 + jh) * 128:(j0 + jh + n) * 128, :].rearrange("(j p) n -> p j n", p=128),
                          (n, D), ("wdn", wi, jh // 2))
            for t in range(ntile):
                ai = (gi * ntile + t) % 2
                for j in range(G):
                    kg = next_pb(0, 4)
                    ku = next_pb(0, 4)
                    for s, k in ((0, kg), (1, ku)):
                        for c in range(8):
                            P.op("pe", lambda e, k=k, c=c, s=s, j=j, t=t, wi=wi: e.matmul(
                                pb[k][:], lhsT=wup[wi][:, c, s, j * 128:(j + 1) * 128],
                                rhs=hnT[:, c, t * 512:(t + 1) * 512], start=(c == 0), stop=(c == 7)),
                                reads=[("wup", wi, s, c // 4)] + [("hnT", 4 * t + b) for b in range(4)],
                                writes=[("pb", k)])
                    si = j % 2
                    P.op("act", lambda e, kg=kg, si=si: e.activation(out=sg[si][:], in_=pb[kg][:], func=AF.Silu),
                         reads=[("pb", kg)], writes=[("sg", si)])
                    P.op("dve", lambda e, ku=ku, si=si, ai=ai, j=j: e.tensor_tensor(
                        out=actT[ai][:, j, :], in0=pb[ku][:], in1=sg[si][:], op=ALU.mult),
                        reads=[("pb", ku), ("sg", si)], writes=[("actT", ai, j)])
                for b in range(4):
                    bb = 4 * t + b
                    for h in range(2):
                        k = 4 + next_pb(0, 3)
                        for j in range(G):
                            P.op("pe", lambda e, k=k, j=j, b=b, h=h, ai=ai, wi=wi, G=G: e.matmul(
                                pb[k][:], lhsT=actT[ai][:, j, b * 128:(b + 1) * 128],
                                rhs=wdn[wi][:, j, h * 512:(h + 1) * 512], start=(j == 0), stop=(j == G - 1)),
                                reads=[("actT", ai, j), ("wdn", wi, j // 2)], writes=[("pb", k)])
                        P.op("dve", lambda e, k=k, bb=bb, h=h: e.tensor_tensor(
                            out=xres[:, bb, h * 512:(h + 1) * 512], in0=pb[k][:],
                            in1=xres[:, bb, h * 512:(h + 1) * 512], op=ALU.add),
                            reads=[("pb", k)], writes=[("xres", bb)])
        P.dma("sp", "yout", [(y[t0:t0 + unit, :].rearrange("(b p) d -> p b d", p=128), xres[:])],
              reads=[("xres", bb) for bb in range(nblk)], writes=[("y", u)])


FM = [("gq", 128), ("gk", 128), ("glr", 16), ("gg0", 96), ("gg1", 96),
      ("sq0", 64), ("sq1", 64), ("sq2", 64), ("sk0", 64), ("sk1", 64), ("sk2", 64),
      ("hq0", 128), ("hq1", 128), ("hf0", 128), ("hf1", 128), ("hgt0", 64), ("hgt1", 64)]
FM_OFF = {}
_o = 0
for _n, _m in FM:
    FM_OFF[_n] = (_o, _m)
    _o += ((_m + 63) // 64) * 64
TM_OFF = _o
WC = _o + 512
CV_BDEC, CV_GLAG, CV_SBQ, CV_SBK, CV_SBO, CV_HGO, CV_LB = 0, 1, 2, 3, 4, 5, 6
CF_R128, CF_R64, CF_TRI, CF_BD64, CF_STRICT = 0, 512, 1024, 1152, 1280
CF_M0, CF_M1, CF_P0, CF_P1 = 1408, 1920, 2432, 2433
CF_W = 2436
CB_NEGINCL, CB_NEGONES, CB_ONES, CB_ZERO, CB_IDENT = 0, 128, 256, 384, 512
CB_W = 640


def make_consts():
    f = np.zeros((128, CF_W), np.float32)
    r = np.ones(512, np.float32); r[::128] = 0; f[:, CF_R128:CF_R128 + 512] = r
    r = np.ones(512, np.float32); r[::64] = 0; f[:, CF_R64:CF_R64 + 512] = r
    j = np.arange(128)[:, None]; i = np.arange(128)[None, :]
    f[:, CF_TRI:CF_TRI + 128] = (j <= i)
    f[:, CF_BD64:CF_BD64 + 128] = (j <= i) & ((j // 64) == (i // 64))
    f[:, CF_STRICT:CF_STRICT + 128] = (j < i)
    cc = np.arange(512)
    f[:, CF_M0:CF_M0 + 512] = ((cc % 128) < 64)
    f[:, CF_M1:CF_M1 + 512] = ((cc % 128) >= 64)
    f[0:64, CF_P0] = 1
    f[64:128, CF_P1] = 1
    b = np.zeros((128, CB_W), np.float32)
    b[:, CB_NEGINCL:CB_NEGINCL + 128] = -(j >= i).astype(np.float32)
    b[:, CB_NEGONES:CB_NEGONES + 128] = -1
    b[:, CB_ONES:CB_ONES + 128] = 1
    b[:, CB_IDENT:CB_IDENT + 128] = np.eye(128)
    return f, b.astype(NPBF)


def build_mixer(T, li, parts="sgh"):
    nc = bass.Bass("TRN2", target_bir_lowering=False)
    dr = {}
    dr["x"] = nc.dram_tensor("x", [T, D], F32, kind="ExternalInput").ap()
    dr["w_in"] = nc.dram_tensor("w_in", [D, WC], F32, kind="ExternalInput").ap()
    dr["g_bc"] = nc.dram_tensor("g_bc", [128, D], F32, kind="ExternalInput").ap()
    dr["cvec"] = nc.dram_tensor("cvec", [128, 16], F32, kind="ExternalInput").ap()
    dr["wdec"] = nc.dram_tensor("wdec", [16, 128], F32, kind="ExternalInput").ap()
    dr["cf"] = nc.dram_tensor("cf", [128, CF_W], F32, kind="ExternalInput").ap()
    dr["cb"] = nc.dram_tensor("cb", [128, CB_W], BF16, kind="ExternalInput").ap()
    dr["mT"] = nc.dram_tensor("mT", [512, T], BF16, kind="ExternalOutput").ap()
    A = Alloc(nc)
    P = Prog(nc)
    emit_mixer(nc, A, P, dr, T, li, parts)
    P.wait_all("sp", [("mT", t) for t in range(T // 512)])
    P.emit()
    A.close()
    return nc


def emit_mixer(nc, A, P, dr, T, li, parts):
    NTILE = T // 512
    NBLK = T // 128
    win = A.sb("win", [128, 8, WC], BF16)
    stage = [A.sb(f"stage{i}", [128, 1024], F32) for i in range(2)]
    gbc = A.sb("gbc", [128, D], F32)
    cv = A.sb("cv", [128, 16], F32)
    cv2 = A.sb("cv2", [128, 16], F32)
    wdec_f = A.sb("wdec_f", [16, 128], F32)
    wdec = A.sb("wdec", [16, 128], BF16)
    cf = A.sb("cf", [128, CF_W], F32)
    cb = A.sb("cb", [128, CB_W], BF16)
    xt = A.sb("xt", [128, 4, D], F32)
    sq = A.sb("sq", [128, D], BF16)
    stat = A.sb("stat", [128, 16], F32)
    hn = [A.sb(f"hn{i}", [128, D], BF16) for i in range(2)]
    hT = A.sb("hT", [128, 8, 512], BF16)
    KT = A.sb("KT", [64, 3, T], BF16)
    Vall = A.sb("Vall", [128, NBLK, 192], BF16)
    vt = A.sb("vt", [128, 4, 320], BF16)
    qT = A.sb("qT", [64, 3, 512], BF16)
    rawf = [A.sb(f"rawf{i}", [128, 512], F32) for i in range(2)]
    sqb = A.sb("sqb", [128, 512], BF16)
    lnv = A.sb("lnv", [128, 512], F32)
    rstd = A.sb("rstd", [128, 512], F32)
    ef = [A.sb(f"ef{i}", [128, 512], F32) for i in range(2)]
    spb = [A.sb(f"spb{i}", [128, 512], BF16) for i in range(2)]
    Sb = A.sb("Sb", [128, 512], BF16)
    wg = [A.sb(f"wg{i}", [128, 512], BF16) for i in range(2)]
    sgl = [A.sb(f"sgl{i}", [96, 512], F32) for i in range(2)]
    shg = [A.sb(f"shg{i}", [64, 512], F32) for i in range(2)]
    qg = A.sb("qg", [128, 512], F32)
    kg = A.sb("kg", [128, 512], F32)
    glr = A.sb("glr", [16, 512], BF16)
    qh = [A.sb(f"qh{i}", [128, 512], F32) for i in range(2)]
    kh = [A.sb(f"kh{i}", [128, 512], F32) for i in range(2)]
    lf = A.sb("lf", [128, 512], F32)
    hfa = A.sb("hfa", [128, 512], F32)
    hfb = A.sb("hfb", [128, 512], F32)
    Gs = A.sb("Gs", [128, 512], F32)
    dlt = A.sb("dlt", [128, 512], F32)
    E1 = A.sb("E1", [128, 512], F32)
    E2 = A.sb("E2", [128, 512], F32)
    dl = A.sb("dl", [128, 8], F32)
    qd = A.sb("qd", [128, 512], BF16)
    kd = A.sb("kd", [128, 512], BF16)
    ATs = [A.sb(f"ATs{i}", [128, 128], BF16) for i in range(2)]
    qz = [A.sb(f"qz{i}", [128, 512], BF16) for i in range(2)]
    vz = [A.sb(f"vz{i}", [128, 64], BF16) for i in range(2)]
    ktok = [A.sb(f"ktok{i}", [128, 128], BF16) for i in range(2)]
    Sg = A.sb("Sg", [128, 96], F32)
    Sgb = A.sb("Sgb", [128, 96], BF16)
    Sh = [A.sb(f"Sh{i}", [128, 64], F32) for i in range(2)]
    Shb = [A.sb(f"Shb{i}", [128, 64], BF16) for i in range(2)]
    onrm = A.sb("onrm", [128, 512], F32)
    mo = [A.sb("mo0", [128, 7, 512], BF16)] * 2
    pb = [A.ps(f"pb{i}", [128, 512], F32) for i in range(7)]
    ptr = A.ps("ptr", [128, 8, 128], BF16)

    def ones(m):
        return cb[0:m, CB_ONES:CB_ONES + m]

    idn_t = A.sb("idn_t", [128, 128], BF16)
    idn = idn_t[:]

    st_i = [0]

    def load_cast(dst_ap, src_ap, shape, dkey):
        i = st_i[0] % 2
        st_i[0] += 1
        n = int(np.prod(shape))
        sv = stage[i][:, 0:n]
        if len(shape) == 2:
            sv = sv.rearrange("p (a b) -> p a b", a=shape[0])
        P.dma("sp", f"stg{i}", [(sv, src_ap)], writes=[("stage", i)])
        P.op("pool", lambda e: e.tensor_copy(out=dst_ap, in_=sv), reads=[("stage", i)], writes=[dkey])

    P.dma("sp", "const", [(gbc[:], dr["g_bc"]), (cv[:], dr["cvec"]), (wdec_f[:], dr["wdec"]),
                          (cf[:], dr["cf"]), (cb[:], dr["cb"]), (idn_t[:], dr["cb"][:, CB_IDENT:CB_IDENT + 128])],
          writes=["gbc", "cv", "wdec_f", "cf", "cb"])
    for c in range(8):
        for hf in range(3):
            load_cast(win[:, c, hf * 704:(hf + 1) * 704], dr["w_in"][c * 128:(c + 1) * 128, hf * 704:(hf + 1) * 704],
                      (704,), ("win", c))
    P.op("dve", lambda e: e.tensor_copy(out=wdec[:], in_=wdec_f[:]), reads=["wdec_f"], writes=["wdec"])
    P.op("dve", lambda e: e.tensor_scalar(out=cv2[:, 0:1], in0=cv[:, CV_BDEC:CV_BDEC + 1], scalar1=-1.0, scalar2=None,
                                          op0=ALU.mult), reads=["cv"], writes=["cv2a"])
    P.op("dve", lambda e: e.tensor_scalar(out=cv2[:, 1:2], in0=cv[:, CV_SBQ:CV_SBQ + 1], scalar1=0.125, scalar2=None,
                                          op0=ALU.mult), reads=["cv"], writes=["cv2b"])
    if li == 0:
        P.op("dve", lambda e: e.memset(cv2[:, 2:4], 0.0), writes=["cv2c"])
        P.op("dve", lambda e: e.memset(cv2[:, 4:6], 1.0), writes=["cv2d"])
    else:
        P.op("dve", lambda e: e.tensor_tensor(out=cv2[:, 6:8], in0=cv[:, CV_LB:CV_LB + 2], in1=cv[:, CV_LB + 2:CV_LB + 4],
                                              op=ALU.subtract), reads=["cv"], writes=["cv2e"])
        P.op("act", lambda e: e.activation(out=cv2[:, 8:10], in_=cv2[:, 6:8], func=AF.Exp), reads=["cv2e"], writes=["cv2f"])
        P.op("act", lambda e: e.activation(out=cv2[:, 10:12], in_=cv2[:, 6:8], func=AF.Exp, scale=-1.0),
             reads=["cv2e"], writes=["cv2g"])
        P.op("dve", lambda e: e.tensor_scalar(out=cv2[:, 8:12], in0=cv2[:, 8:12], scalar1=1.0, scalar2=None, op0=ALU.add),
             reads=["cv2f", "cv2g"], writes=["cv2h"])
        P.op("dve", lambda e: e.reciprocal(out=cv2[:, 2:4], in_=cv2[:, 8:10]), reads=["cv2h"], writes=["cv2c"])
        P.op("dve", lambda e: e.reciprocal(out=cv2[:, 4:6], in_=cv2[:, 10:12]), reads=["cv2h"], writes=["cv2d"])
    P.op("dve", lambda e: e.memset(mo[0][:], 0.0), writes=[("mo", 0, j) for j in range(7)])
    P.op("dve", lambda e: e.memset(Sg[:], 0.0), writes=["Sg"])
    for h in range(2):
        P.op("dve", lambda e, h=h: e.memset(Sh[h][:], 0.0), writes=[("Sh", h)])

    rr = {"g": 0}

    def bank(kind):
        if kind == "w":
            return 3
        k = rr["g"] % 3
        rr["g"] += 1
        return k

    def headnorm(src, srckeys, M, gain, dst, dstkeys, extra=None, extrakeys=()):
        P.op("act", lambda e: e.activation(out=sqb[0:M, :], in_=src, func=AF.Square), reads=srckeys, writes=["sqb"])
        k = bank("g")
        P.op("pe", lambda e: e.matmul(pb[k][0:M, :], lhsT=ones(M), rhs=sqb[0:M, :], start=True, stop=True),
             reads=["sqb", "cb"], writes=[("pb", k)])
        P.op("act", lambda e: e.activation(out=lnv[0:M, :], in_=pb[k][0:M, :], func=AF.Ln, scale=1.0 / M, bias=EPS),
             reads=[("pb", k)], writes=["lnv"])
        P.op("act", lambda e: e.activation(out=rstd[0:M, :], in_=lnv[0:M, :], func=AF.Exp, scale=-0.5),
             reads=["lnv"], writes=["rstd"])
        if extra is None:
            P.op("dve", lambda e: e.scalar_tensor_tensor(out=dst, in0=src, scalar=gain, in1=rstd[0:M, :],
                                                         op0=ALU.mult, op1=ALU.mult),
                 reads=list(srckeys) + ["rstd", "cv", "cv2b"], writes=dstkeys)
        else:
            P.op("dve", lambda e: e.scalar_tensor_tensor(out=onrm[0:M, :], in0=src, scalar=gain, in1=rstd[0:M, :],
                                                         op0=ALU.mult, op1=ALU.mult),
                 reads=list(srckeys) + ["rstd", "cv"], writes=["onrm"])
            P.op("dve", lambda e: e.tensor_tensor(out=dst, in0=onrm[0:M, :], in1=extra, op=ALU.mult),
                 reads=["onrm"] + list(extrakeys), writes=dstkeys)

    def proj_fm(name):
        off, M = FM_OFF[name]
        k = bank("g")
        for c in range(8):
            P.op("pe", lambda e, c=c: e.matmul(pb[k][0:M, :], lhsT=win[:, c, off:off + M], rhs=hT[:, c, :],
                                               start=(c == 0), stop=(c == 7)),
                 reads=[("win", c), "hT"], writes=[("pb", k)])
        return k, M

    for ti in range(NTILE):
        tt = ti * 512
        mi = 0
        if "hT_in" in dr:
            P.dma("sp", "hTin", [(hT[:], dr["hT_in"][ti].rearrange("p (c t) -> p c t", c=8))], writes=["hT"])
        else:
            P.dma("sp", "xin", [(xt[:], dr["x"][tt:tt + 512, :].rearrange("(b p) d -> p b d", p=128))], writes=["xt"])
        for b in range(4 if "hT_in" not in dr else 0):
            P.op("act", lambda e, b=b: e.activation(out=sq[:], in_=xt[:, b, :], func=AF.Square, accum_out=stat[:, b:b + 1]),
                 reads=["xt"], writes=["sq", ("stat", b)])
            P.op("act", lambda e, b=b: e.activation(out=stat[:, 4 + b:5 + b], in_=stat[:, b:b + 1], func=AF.Ln,
                                                    scale=1.0 / D, bias=EPS), reads=[("stat", b)], writes=[("stat2", b)])
            P.op("act", lambda e, b=b: e.activation(out=stat[:, 8 + b:9 + b], in_=stat[:, 4 + b:5 + b], func=AF.Exp,
                                                    scale=-0.5), reads=[("stat2", b)], writes=[("stat3", b)])
            hi = b % 2
            P.op("dve", lambda e, b=b, hi=hi: e.scalar_tensor_tensor(out=hn[hi][:], in0=xt[:, b, :],
                                                                     scalar=stat[:, 8 + b:9 + b], in1=gbc[:],
                                                                     op0=ALU.mult, op1=ALU.mult),
                 reads=["xt", ("stat3", b), "gbc"], writes=[("hn", hi)])
            for c in range(8):
                P.op("pe", lambda e, c=c, hi=hi: e.transpose(ptr[:, c, :], hn[hi][:, c * 128:(c + 1) * 128], idn),
                     reads=[("hn", hi), "cb"], writes=["ptr"])
            P.op("act", lambda e, b=b: e.activation(out=hT[:, :, b * 128:(b + 1) * 128], in_=ptr[:], func=AF.Copy),
                 reads=["ptr"], writes=["hT"])
        if "hT_out" in dr:
            P.dma("pool", "hTout", [(dr["hT_out"][ti].rearrange("p (c t) -> p c t", c=8), hT[:])], reads=["hT"],
                  writes=[("hTs", ti)])
        for b in range(4):
            k = bank("g")
            for c in range(8):
                P.op("pe", lambda e, c=c, b=b, k=k: e.matmul(pb[k][:], lhsT=hT[:, c, b * 128:(b + 1) * 128],
                                                             rhs=win[:, c, TM_OFF:TM_OFF + 512], start=(c == 0), stop=(c == 7)),
                     reads=[("win", c), "hT"], writes=[("pb", k)])
            P.op("act", lambda e, b=b, k=k, ti=ti: e.activation(out=Vall[:, ti * 4 + b, :], in_=pb[k][:, 0:192], func=AF.Copy),
                 reads=[("pb", k)], writes=[("Vall", ti * 4 + b)])
            P.op("act", lambda e, b=b, k=k: e.activation(out=vt[:, b, :], in_=pb[k][:, 192:512], func=AF.Copy),
                 reads=[("pb", k)], writes=[("vt", b)])
        for h in range(2):
            k, M = proj_fm(f"gg{h}")
            P.op("act", lambda e, h=h, k=k: e.activation(out=sgl[h][:], in_=pb[k][0:96, :], func=AF.Silu),
                 reads=[("pb", k)], writes=[("sgl", h)])
        for h in range(2):
            k, M = proj_fm(f"hgt{h}")
            P.op("act", lambda e, h=h, k=k: e.activation(out=shg[h][:], in_=pb[k][0:64, :], func=AF.Silu),
                 reads=[("pb", k)], writes=[("shg", h)])

        sb_steps = []
        if "s" in parts:
            for h in range(3):
                k, M = proj_fm(f"sk{h}")
                ri = h % 2
                P.op("act", lambda e, k=k, ri=ri: e.activation(out=rawf[ri][0:64, :], in_=pb[k][0:64, :], func=AF.Copy),
                     reads=[("pb", k)], writes=[("rawf", ri)])
                headnorm(rawf[ri][0:64, :], [("rawf", ri)], 64, cv[0:64, CV_SBK:CV_SBK + 1],
                         KT[:, h, tt:tt + 512], [("KT", h, ti)])
                k, M = proj_fm(f"sq{h}")
                ri = (h + 1) % 2
                P.op("act", lambda e, k=k, ri=ri: e.activation(out=rawf[ri][0:64, :], in_=pb[k][0:64, :], func=AF.Copy),
                     reads=[("pb", k)], writes=[("rawf", ri)])
                headnorm(rawf[ri][0:64, :], [("rawf", ri)], 64, cv2[0:64, 1:2], qT[:, h, :], [("qT", h)])
            nkb = 4 * ti + 4
            pairs = [(h, idx, kb) for h in range(3) for idx, kb in enumerate(range(nkb - 1, -1, -1))]
            info = {}

            def stageA(n):
                h, idx, kb = pairs[n]
                r = kb - 4 * ti
                c0 = 128 * r if r >= 0 else 0
                ei = n % 2
                ktile = kb // 4
                kz = bank("z")
                info[n] = (c0, ei, r)
                P.op("pe", lambda e: e.matmul(pb[kz][:, c0:512], lhsT=KT[:, h, kb * 128:(kb + 1) * 128], rhs=qT[:, h, c0:512],
                                              start=True, stop=True),
                     reads=[("KT", h, ktile), ("qT", h)], writes=[("pb", kz)])
                P.op("act", lambda e: e.activation(out=ef[ei][:, c0:512], in_=pb[kz][:, c0:512], func=AF.Exp),
                     reads=[("pb", kz)], writes=[("ef", ei)])
                P.op("act", lambda e: e.activation(out=spb[ei][:, c0:512], in_=ef[ei][:, c0:512], func=AF.Ln, bias=1.0),
                     reads=[("ef", ei)], writes=[("spb", ei)])
                if r >= 0:
                    P.op("dve", lambda e: e.tensor_tensor(out=spb[ei][:, c0:c0 + 128], in0=spb[ei][:, c0:c0 + 128],
                                                          in1=cf[:, CF_STRICT:CF_STRICT + 128], op=ALU.mult),
                         reads=[("spb", ei), "cf"], writes=[("spb", ei)])

            def stageB1(n):
                h, idx, kb = pairs[n]
                c0, ei, r = info[n]
                first = (idx == 0)
                last = (kb == 0)
                ktile = kb // 4
                kw = bank("w")
                P.op("pe", lambda e: e.matmul(pb[kw][:, c0:512], lhsT=KT[:, h, kb * 128:(kb + 1) * 128], rhs=qT[:, h, c0:512],
                                              start=True, stop=False),
                     reads=[("KT", h, ktile), ("qT", h)], writes=[("pb", kw)])
                P.op("pe", lambda e: e.matmul(pb[kw][:, c0:512], lhsT=cb[:, CB_NEGINCL:CB_NEGINCL + 128], rhs=spb[ei][:, c0:512],
                                              start=False, stop=first),
                     reads=[("spb", ei), "cb"], writes=[("pb", kw)])
                if not first:
                    P.op("pe", lambda e: e.matmul(pb[kw][:, c0:512], lhsT=cb[:, CB_NEGONES:CB_NEGONES + 128], rhs=Sb[:, c0:512],
                                                  start=False, stop=True),
                         reads=["Sb", "cb"], writes=[("pb", kw)])
                P.op("act", lambda e: e.activation(out=wg[ei][:, c0:512], in_=pb[kw][:, c0:512], func=AF.Exp),
                     reads=[("pb", kw)], writes=[("wg", ei)])
                if r >= 0:
                    P.op("dve", lambda e: e.tensor_tensor(out=wg[ei][:, c0:c0 + 128], in0=wg[ei][:, c0:c0 + 128],
                                                          in1=cf[:, CF_STRICT:CF_STRICT + 128], op=ALU.mult),
                         reads=[("wg", ei), "cf"], writes=[("wg", ei)])
                if not last:
                    if first:
                        P.op("dve", lambda e: e.memset(Sb[:], 0.0), writes=["Sb"])
                    P.op("dve", lambda e: e.tensor_tensor(out=Sb[:, c0:512], in0=Sb[:, c0:512], in1=spb[ei][:, c0:512], op=ALU.add),
                         reads=[("spb", ei), "Sb"], writes=["Sb"])

            def stageB2(n):
                h, idx, kb = pairs[n]
                c0, ei, r = info[n]
                first = (idx == 0)
                last = (kb == 0)
                if first:
                    P.op("pe", lambda e: e.matmul(pb[6][0:64, :], lhsT=cb[:, CB_ZERO:CB_ZERO + 64], rhs=cb[:, 0:512],
                                                  start=True, stop=False), reads=["cb"], writes=[("pb", 6)])
                P.op("pe", lambda e: e.matmul(pb[6][0:64, c0:512], lhsT=Vall[:, kb, h * 64:(h + 1) * 64], rhs=wg[ei][:, c0:512],
                                              start=False, stop=last),
                     reads=[("wg", ei), ("Vall", kb)], writes=[("pb", 6)])
                if last:
                    headnorm(pb[6][0:64, :], [("pb", 6)], 64, cv[0:64, CV_SBO:CV_SBO + 1], mo[mi][0:64, 2 + h, :],
                             [("mo", mi, 2 + h)])

            npairs = len(pairs)
            sb_steps.append(lambda: stageA(0))
            for n in range(npairs):
                if n + 1 < npairs:
                    sb_steps.append(lambda n=n: stageA(n + 1))
                sb_steps.append(lambda n=n: stageB1(n))
                if n >= 1:
                    sb_steps.append(lambda n=n: stageB2(n - 1))
            sb_steps.append(lambda: stageB2(npairs - 1))

        def hg_gen():
            for h in range(2):
                k, M = proj_fm(f"hq{h}")
                P.op("act", lambda e, k=k, h=h: e.activation(out=qh[h][:], in_=pb[k][:], func=AF.Copy),
                     reads=[("pb", k)], writes=[("qh", h)])
                k, M = proj_fm(f"hf{h}")
                P.op("act", lambda e, k=k: e.activation(out=hfa[:], in_=pb[k][:], func=AF.Exp, scale=-1.0),
                     reads=[("pb", k)], writes=["hfa"])
                yield
                P.op("dve", lambda e: e.tensor_scalar(out=hfa[:], in0=hfa[:], scalar1=1.0, scalar2=None, op0=ALU.add),
                     reads=["hfa"], writes=["hfa"])
                P.op("dve", lambda e: e.reciprocal(out=hfb[:], in_=hfa[:]), reads=["hfa"], writes=["hfb"])
                P.op("act", lambda e, h=h: e.activation(out=hfb[:], in_=hfb[:], func=AF.Identity, scale=cv2[:, 4 + h:5 + h],
                                                        bias=cv2[:, 2 + h:3 + h]),
                     reads=["hfb", "cv2c", "cv2d"], writes=["hfb"])
                P.op("act", lambda e: e.activation(out=lf[:], in_=hfb[:], func=AF.Ln), reads=["hfb"], writes=["lf"])
                P.op("act", lambda e, h=h: e.activation(out=kh[h][:], in_=hfb[:], func=AF.Identity, scale=-1.0, bias=1.0),
                     reads=["hfb"], writes=[("kh", h)])
                yield
                yield from lin_attn(P, L_, "hg", ti, mi, h)

        def gla_gen():
            k, M = proj_fm("gq")
            P.op("act", lambda e, k=k: e.activation(out=qg[:], in_=pb[k][:], func=AF.Copy, scale=48.0 ** -0.5),
                 reads=[("pb", k)], writes=["qg"])
            k, M = proj_fm("gk")
            P.op("act", lambda e, k=k: e.activation(out=kg[:], in_=pb[k][:], func=AF.Copy), reads=[("pb", k)], writes=["kg"])
            yield
            k, M = proj_fm("glr")
            P.op("act", lambda e, k=k: e.activation(out=glr[:], in_=pb[k][0:16, :], func=AF.Copy), reads=[("pb", k)], writes=["glr"])
            k = bank("g")
            P.op("pe", lambda e, k=k: e.matmul(pb[k][:], lhsT=wdec[:], rhs=glr[:], start=True, stop=True),
                 reads=["wdec", "glr"], writes=[("pb", k)])
            P.op("act", lambda e, k=k: e.activation(out=hfa[:], in_=pb[k][:], func=AF.Exp, scale=-1.0, bias=cv2[:, 0:1]),
                 reads=[("pb", k), "cv2a"], writes=["hfa"])
            P.op("act", lambda e: e.activation(out=lf[:], in_=hfa[:], func=AF.Ln, bias=1.0), reads=["hfa"], writes=["lf"])
            yield
            yield from lin_attn(P, L_, "gla", ti, mi)

        def bg_gen():
            if "h" in parts:
                yield from hg_gen()
            if "g" in parts:
                yield from gla_gen()

        L_ = dict(locals())
        bg = bg_gen()
        n_bg = 2 * 11 + 8
        every = max(1, len(sb_steps) // n_bg) if sb_steps else 1
        for i, st in enumerate(sb_steps):
            st()
            if i % every == every - 1:
                next(bg, None)
        for _ in bg:
            pass

        outs = []
        rows = [(0, 96), (96, 96), (192, 64), (256, 64), (320, 64), (384, 64), (448, 64)]
        for j, (r0, m) in enumerate(rows):
            outs.append((dr["mT"][r0:r0 + m, tt:tt + 512], mo[mi][0:m, j, :]))
        P.dma("pool", f"mout{mi}", outs, reads=[("mo", mi, j) for j in range(7)], writes=[("mT", ti)])


def lin_attn(P, L, kind, ti, mi, h=0):
    cf, cb, pb, ptr = L["cf"], L["cb"], L["pb"], L["ptr"]
    Gs, dlt, E1, E2, dl, qd, kd = L["Gs"], L["dlt"], L["E1"], L["E2"], L["dl"], L["qd"], L["kd"]
    ATs, ktok, vt, lf = L["ATs"], L["ktok"], L["vt"], L["lf"]
    bank, headnorm, mo, cv = L["bank"], L["headnorm"], L["mo"], L["cv"]
    idn = L["idn"]
    if kind == "gla":
        C, nb, s1, s2 = 128, 4, 1.0 / 16, -1.0 / 16
        q, k, qkeys, kkeys = L["qg"], L["kg"], ["qg"], ["kg"]
        roff, mask = CF_R128, CF_TRI
    else:
        C, nb, s1, s2 = 64, 8, -1.0, 1.0
        q, k, qkeys, kkeys = L["qh"][h], L["kh"][h], [("qh", h)], [("kh", h)]
        roff, mask = CF_R64, CF_BD64
    P.op("dve", lambda e: e.tensor_tensor_scan(out=Gs[:], data0=cf[:, roff:roff + 512], data1=lf[:], initial=0.0,
                                               op0=ALU.mult, op1=ALU.add), reads=["lf", "cf"], writes=["Gs"])
    gv = Gs[:].rearrange("p (b c) -> p b c", c=C)
    P.op("dve", lambda e: e.tensor_tensor(out=dlt[:].rearrange("p (b c) -> p b c", c=C),
                                          in0=gv[:, :, C - 1:C].to_broadcast([128, nb, C]), in1=gv, op=ALU.subtract),
         reads=["Gs"], writes=["dlt"])
    P.op("act", lambda e: e.activation(out=E1[:], in_=dlt[:], func=AF.Exp, scale=s1), reads=["dlt"], writes=["E1"])
    P.op("act", lambda e: e.activation(out=E2[:], in_=dlt[:], func=AF.Exp, scale=-s1), reads=["dlt"], writes=["E2"])
    P.op("act", lambda e: e.activation(out=dl[:, 0:nb], in_=gv[:, :, C - 1], func=AF.Exp, scale=s2), reads=["Gs"], writes=["dl"])
    P.op("dve", lambda e: e.tensor_tensor(out=qd[:], in0=q[:], in1=E1[:], op=ALU.mult), reads=qkeys + ["E1"], writes=["qd"])
    P.op("dve", lambda e: e.tensor_tensor(out=kd[:], in0=k[:], in1=E2[:], op=ALU.mult), reads=kkeys + ["E2"], writes=["kd"])
    if kind != "gla":
        for s in range(2):
            P.op("dve", lambda e, s=s: e.tensor_tensor(out=L["qz"][s][:], in0=qd[:], in1=cf[:, CF_M0 + 512 * s:CF_M0 + 512 * (s + 1)], op=ALU.mult),
                 reads=["qd", "cf"], writes=[("qz", s)])
    yield
    for b4 in range(4):
        cs = slice(b4 * 128, (b4 + 1) * 128)
        ai = b4 % 2
        P.op("pe", lambda e, cs=cs: e.transpose(ptr[:, 0, :], kd[:, cs], idn), reads=["kd", "cb"], writes=["ptr"])
        P.op("act", lambda e, ai=ai: e.activation(out=ktok[ai][:], in_=ptr[:, 0, :], func=AF.Copy), reads=["ptr"], writes=[("ktok", ai)])
        if kind == "gla":
            Sg, Sgb = L["Sg"], L["Sgb"]
            P.op("dve", lambda e, b4=b4: e.tensor_scalar(out=Sgb[:], in0=Sg[:], scalar1=dl[:, b4:b4 + 1], scalar2=None, op0=ALU.mult),
                 reads=["Sg", "dl"], writes=["Sgb"])
            ku = bank("w")
            for hh in range(2):
                ps = slice(64 * hh, 64 * hh + 64)
                ka = bank("g")
                P.op("pe", lambda e, ka=ka, ps=ps, cs=cs: e.matmul(pb[ka][:, 0:128], lhsT=kd[ps, cs], rhs=qd[ps, cs], start=True, stop=True),
                     reads=["kd", "qd"], writes=[("pb", ka)])
                P.op("dve", lambda e, ka=ka, hh=hh: e.tensor_tensor(out=ATs[hh][:], in0=pb[ka][:, 0:128], in1=cf[:, mask:mask + 128], op=ALU.mult),
                     reads=[("pb", ka), "cf"], writes=[("ATs", hh)])
                acc = 6
                P.op("pe", lambda e, hh=hh, b4=b4, cs=cs: e.matmul(pb[4 + hh][0:96, cs],
                                                                   lhsT=vt[:, b4, hh * 96:(hh + 1) * 96], rhs=ATs[hh][:], start=True, stop=False),
                     reads=[("ATs", hh), ("vt", b4)], writes=[("pb", 4 + hh)])
                P.op("pe", lambda e, hh=hh, ps=ps, cs=cs: e.matmul(pb[4 + hh][0:96, cs],
                                                                   lhsT=Sgb[ps, :], rhs=qd[ps, cs], start=False, stop=True),
                     reads=["Sgb", "qd"], writes=[("pb", 4 + hh)])
                P.op("pe", lambda e, hh=hh, ps=ps, b4=b4, ai=ai, ku=ku: e.matmul(pb[ku][ps, 0:96], lhsT=ktok[ai][:, ps],
                                                                                rhs=vt[:, b4, hh * 96:(hh + 1) * 96], start=True, stop=True),
                     reads=[("ktok", ai), ("vt", b4)], writes=[("pb", ku)])
            P.op("dve", lambda e, b4=b4, ku=ku: e.scalar_tensor_tensor(out=Sg[:], in0=Sg[:], scalar=dl[:, b4:b4 + 1], in1=pb[ku][:, 0:96],
                                                                      op0=ALU.mult, op1=ALU.add),
                 reads=["Sg", "dl", ("pb", ku)], writes=["Sg"])
            yield
        else:
            Sh, Shb = L["Sh"][h], L["Shb"][h]
            ka = bank("g")
            P.op("pe", lambda e, ka=ka, cs=cs: e.matmul(pb[ka][:, 0:128], lhsT=kd[:, cs], rhs=qd[:, cs], start=True, stop=True),
                 reads=["kd", "qd"], writes=[("pb", ka)])
            P.op("dve", lambda e, ka=ka: e.tensor_tensor(out=ATs[0][:], in0=pb[ka][:, 0:128], in1=cf[:, mask:mask + 128], op=ALU.mult),
                 reads=[("pb", ka), "cf"], writes=[("ATs", 0)])
            P.op("pe", lambda e, b4=b4, cs=cs: e.matmul(pb[4][0:64, cs], lhsT=vt[:, b4, 192 + h * 64:192 + (h + 1) * 64], rhs=ATs[0][:],
                                                        start=True, stop=False),
                 reads=[("ATs", 0), ("vt", b4)], writes=[("pb", 4)])
            for s in range(2):
                blk = 2 * b4 + s
                P.op("dve", lambda e, s=s, b4=b4: e.tensor_scalar(out=L["vz"][s][:], in0=vt[:, b4, 192 + h * 64:192 + (h + 1) * 64],
                                                               scalar1=cf[:, CF_P0 + s:CF_P0 + s + 1], scalar2=None, op0=ALU.mult),
                     reads=[("vt", b4), "cf"], writes=[("vz", s)])
                P.op("dve", lambda e, blk=blk: e.tensor_scalar(out=Shb[:], in0=Sh[:], scalar1=dl[:, blk:blk + 1], scalar2=None, op0=ALU.mult),
                     reads=[("Sh", h), "dl"], writes=[("Shb", h)])
                P.op("pe", lambda e, cs=cs, s=s: e.matmul(pb[4][0:64, cs], lhsT=Shb[:], rhs=L["qz"][s][:, cs], start=False, stop=(s == 1)),
                     reads=[("Shb", h), ("qz", s)], writes=[("pb", 4)])
                ku = bank("g")
                P.op("pe", lambda e, ku=ku, s=s, ai=ai: e.matmul(pb[ku][:, 0:64], lhsT=ktok[ai][:], rhs=L["vz"][s][:], start=True, stop=True),
                     reads=[("ktok", ai), ("vz", s)], writes=[("pb", ku)])
                P.op("dve", lambda e, blk=blk, ku=ku: e.scalar_tensor_tensor(out=Sh[:], in0=Sh[:], scalar=dl[:, blk:blk + 1], in1=pb[ku][:, 0:64],
                                                                            op0=ALU.mult, op1=ALU.add),
                     reads=[("Sh", h), "dl", ("pb", ku)], writes=[("Sh", h)])
                yield
    if kind == "gla":
        for hh in range(2):
            src = pb[4 + hh][0:96, :]
            headnorm(src, [("pb", 4 + hh)], 96, cv[0:96, CV_GLAG:CV_GLAG + 1], mo[mi][0:96, hh, :], [("mo", mi, hh)],
                     extra=L["sgl"][hh][:], extrakeys=[("sgl", hh)])
    else:
        headnorm(pb[4][0:64, :], [("pb", 4)], 64, cv[0:64, CV_HGO:CV_HGO + 1], mo[mi][0:64, 5 + h, :], [("mo", mi, 5 + h)],
                 extra=L["shg"][h][:], extrakeys=[("shg", h)])


def mixer_rows(hh):
    rows = []
    for j in range(2):
        rows.append((96 * j, 96, (2 * hh + j) * 96))
    for j in range(3):
        rows.append((192 + 64 * j, 64, 384 + (3 * hh + j) * 64))
    for j in range(2):
        rows.append((384 + 64 * j, 64, 768 + (2 * hh + j) * 64))
    return rows


_CONSTS = None


def mixer_inputs(p, li, hh, xb):
    global _CONSTS
    if _CONSTS is None:
        _CONSTS = make_consts()
    W = p["w_in"][li]
    offs = np.concatenate([[0], np.cumsum([192, 192, 384, 16, 384, 384, 384, 384, 512, 512, 256, 256])])
    (o_gq, o_gk, o_gv, o_lr, o_gg, o_sq, o_sk, o_sv, o_hq, o_hf, o_hi, o_hg) = offs[:12]
    wc = np.zeros((D, WC), np.float32)

    def put(name, src0, n, dst_off=0):
        off, M = FM_OFF[name]
        wc[:, off + dst_off:off + dst_off + n] = W[:, src0:src0 + n]

    for j in range(2):
        hd = 2 * hh + j
        put("gq", o_gq + hd * 48, 48, 64 * j)
        put("gk", o_gk + hd * 48, 48, 64 * j)
        put(f"gg{j}", o_gg + hd * 96, 96)
        put(f"hq{j}", o_hq + hd * 128, 128)
        put(f"hf{j}", o_hf + hd * 128, 128)
        put(f"hgt{j}", o_hg + hd * 64, 64)
        wc[:, TM_OFF + 192 + 96 * j:TM_OFF + 192 + 96 * (j + 1)] = W[:, o_gv + hd * 96:o_gv + (hd + 1) * 96]
        wc[:, TM_OFF + 384 + 64 * j:TM_OFF + 384 + 64 * (j + 1)] = W[:, o_hi + hd * 64:o_hi + (hd + 1) * 64]
    put("glr", o_lr, 16)
    for j in range(3):
        hd = 3 * hh + j
        put(f"sq{j}", o_sq + hd * 64, 64)
        put(f"sk{j}", o_sk + hd * 64, 64)
        wc[:, TM_OFF + 64 * j:TM_OFF + 64 * (j + 1)] = W[:, o_sv + hd * 64:o_sv + (hd + 1) * 64]
    cvec = np.zeros((128, 16), np.float32)
    wdec = np.zeros((16, 128), np.float32)
    for j in range(2):
        hd = 2 * hh + j
        cvec[64 * j:64 * j + 48, CV_BDEC] = p["gla_b_decay"][li][hd * 48:(hd + 1) * 48]
        wdec[:, 64 * j:64 * j + 48] = p["gla_w_decay"][li][:, hd * 48:(hd + 1) * 48]
        cvec[:, CV_LB + j] = p["hg_lb_logits"][0][hd * 128:(hd + 1) * 128]
        cvec[:, CV_LB + 2 + j] = p["hg_lb_logits"][min(1, p["hg_lb_logits"].shape[0] - 1)][hd * 128:(hd + 1) * 128]
    cvec[0:96, CV_GLAG] = p["gla_out_g"][li]
    cvec[0:64, CV_SBQ] = p["sb_q_g"][li]
    cvec[0:64, CV_SBK] = p["sb_k_g"][li]
    cvec[0:64, CV_SBO] = p["sb_out_g"][li]
    cvec[0:64, CV_HGO] = p["hg_out_g"][li]
    return dict(x=np.ascontiguousarray(xb), w_in=wc,
                g_bc=np.ascontiguousarray(np.broadcast_to(p["norm_mix_g"][li], (128, D))),
                cvec=cvec, wdec=wdec, cf=_CONSTS[0], cb=_CONSTS[1])


_NC_CACHE = {}


def _get(kind, *args):
    key = (kind,) + args
    if key not in _NC_CACHE:
        _NC_CACHE[key] = build_mixer(*args) if kind == "mix" else build_ffn(*args)
    return _NC_CACHE[key]


def kernel_unfused(**inp):
    p = {k: np.asarray(v) for k, v in inp.items()}
    x = np.ascontiguousarray(p["x"], dtype=np.float32)
    B, T, _ = x.shape
    depth = p["w_in"].shape[0]
    ident = np.eye(128, dtype=np.float32).astype(NPBF)
    cores = list(range(8))
    for li in range(depth):
        nc = _get("mix", T, li, "sgh")
        in_maps = [mixer_inputs(p, li, c % 2, x[c // 2]) for c in cores]
        res = run_bass_kernel_spmd(nc, in_maps, core_ids=cores).results
        mT = np.zeros((B, D, T), NPBF)
        for c in cores:
            b, hh = c // 2, c % 2
            m = res[c]["mT"]
            for (r0, n, g0) in mixer_rows(hh):
                mT[b, g0:g0 + n, :] = m[r0:r0 + n, :]
        nc2 = _get("ffn", T // 2)
        in_maps = []
        for c in cores:
            b, th = c // 2, c % 2
            sl = slice(th * (T // 2), (th + 1) * (T // 2))
            in_maps.append(dict(x=np.ascontiguousarray(x[b, sl]), mT=np.ascontiguousarray(mT[b][:, sl]),
                                w_out=p["w_out"][li], w_up=p["w_ffn_up"][li], w_down=p["w_ffn_down"][li],
                                g_bc=np.ascontiguousarray(np.broadcast_to(p["norm_ffn_g"][li], (128, D))), ident=ident))
        res = run_bass_kernel_spmd(nc2, in_maps, core_ids=cores).results
        xn = np.empty_like(x)
        for c in cores:
            b, th = c // 2, c % 2
            xn[b, th * (T // 2):(th + 1) * (T // 2)] = res[c]["y"]
        x = xn
    return x


def build_fused(T, depth=2):
    nc = bass.Bass("TRN2", target_bir_lowering=False)
    x = nc.dram_tensor("x", [T, D], F32, kind="ExternalInput").ap()
    w_in = nc.dram_tensor("w_in", [depth, 2, D, WC], F32, kind="ExternalInput").ap()
    gmix = nc.dram_tensor("gmix", [depth, 128, D], F32, kind="ExternalInput").ap()
    gffn = nc.dram_tensor("gffn", [depth, 128, D], F32, kind="ExternalInput").ap()
    cvec = nc.dram_tensor("cvec", [depth, 2, 128, 16], F32, kind="ExternalInput").ap()
    wdec = nc.dram_tensor("wdec", [depth, 2, 16, 128], F32, kind="ExternalInput").ap()
    cf = nc.dram_tensor("cf", [128, CF_W], F32, kind="ExternalInput").ap()
    cb = nc.dram_tensor("cb", [128, CB_W], BF16, kind="ExternalInput").ap()
    w_out = nc.dram_tensor("w_out", [depth, D, D], F32, kind="ExternalInput").ap()
    w_up = nc.dram_tensor("w_up", [depth, D, 2 * DFF], F32, kind="ExternalInput").ap()
    w_down = nc.dram_tensor("w_down", [depth, DFF, D], F32, kind="ExternalInput").ap()
    y = nc.dram_tensor("y", [T, D], F32, kind="ExternalOutput").ap()
    mTs = nc.dram_tensor("mTs", [D, T], BF16).ap()
    hTs = nc.dram_tensor("hTs", [T // 512, 128, 8 * 512], BF16).ap()
    xs = nc.dram_tensor("xs", [T, D], F32).ap()
    unit = min(UNIT, T)
    for li in range(depth):
        xin = x if li == 0 else xs
        for hh in range(2):
            A = Alloc(nc)
            P = Prog(nc)
            dr = dict(x=xin, w_in=w_in[li, hh], g_bc=gmix[li], cvec=cvec[li, hh], wdec=wdec[li, hh], cf=cf, cb=cb,
                      mT=mTs[hh * 512:(hh + 1) * 512, :])
            dr["hT_out" if hh == 0 else "hT_in"] = hTs
            emit_mixer(nc, A, P, dr, T, li, "sgh")
            P.wait_all("sp", [("mT", t) for t in range(T // 512)] + ([("hTs", t) for t in range(T // 512)] if hh == 0 else []))
            P.emit()
            A.close()
        A = Alloc(nc)
        P = Prog(nc)
        emit_ffn(nc, A, P, xin, mTs, w_out[li], w_up[li], w_down[li], gffn[li], cb[:, CB_IDENT:CB_IDENT + 128],
                 (y if li == depth - 1 else xs), T, unit, T // unit)
        P.wait_all("sp", [("y", u) for u in range(T // unit)])
        P.emit()
        A.close()
    return nc


def fused_inputs(p, b):
    global _CONSTS
    if _CONSTS is None:
        _CONSTS = make_consts()
    depth = p["w_in"].shape[0]
    w_in = np.zeros((depth, 2, D, WC), np.float32)
    cvec = np.zeros((depth, 2, 128, 16), np.float32)
    wdec = np.zeros((depth, 2, 16, 128), np.float32)
    w_out = np.zeros((depth, D, D), np.float32)
    for li in range(depth):
        for hh in range(2):
            m = mixer_inputs(p, li, hh, p["x"][b])
            w_in[li, hh] = m["w_in"]
            cvec[li, hh] = m["cvec"]
            wdec[li, hh] = m["wdec"]
            for (r0, n, g0) in mixer_rows(hh):
                w_out[li, hh * 512 + r0:hh * 512 + r0 + n, :] = p["w_out"][li][g0:g0 + n, :]
    return dict(x=np.ascontiguousarray(p["x"][b]), w_in=w_in,
                gmix=np.ascontiguousarray(np.broadcast_to(p["norm_mix_g"][:, None, :], (depth, 128, D))),
                gffn=np.ascontiguousarray(np.broadcast_to(p["norm_ffn_g"][:, None, :], (depth, 128, D))),
                cvec=cvec, wdec=wdec, cf=_CONSTS[0], cb=_CONSTS[1], w_out=w_out,
                w_up=np.ascontiguousarray(p["w_ffn_up"]), w_down=np.ascontiguousarray(p["w_ffn_down"]))


def kernel(**inp):
    p = {k: np.asarray(v) for k, v in inp.items()}
    p["x"] = np.ascontiguousarray(p["x"], dtype=np.float32)
    B, T, _ = p["x"].shape
    nc = _get_fused(T, p["w_in"].shape[0])
    cores = list(range(8))
    per_b = [fused_inputs(p, b) for b in range(B)]
    in_maps = [per_b[c % B] for c in cores]
    res = run_bass_kernel_spmd(nc, in_maps, core_ids=cores).results
    return np.stack([res[b]["y"] for b in range(B)], axis=0)


def _get_fused(T, depth):
    key = ("fused", T, depth)
    if key not in _NC_CACHE:
        _NC_CACHE[key] = build_fused(T, depth)
    return _NC_CACHE[key]
```

```python
import numpy as np
import ml_dtypes
import concourse.bass as bass
import concourse.mybir as mybir
from concourse.bass_utils import run_bass_kernel_spmd

F32 = mybir.dt.float32
BF16 = mybir.dt.bfloat16
AF = mybir.ActivationFunctionType
ALU = mybir.AluOpType
AX = mybir.AxisListType
NPBF = ml_dtypes.bfloat16

D = 1024
DFF = 2816
EPS = 1e-6


class Prog:
    ENGS = ("pe", "act", "dve", "pool", "sp")

    PHASE = [0]

    def __init__(self, nc, same_eng_sync=True):
        self.nc = nc
        Prog.PHASE[0] += 1
        self.pfx = f"p{Prog.PHASE[0]}_"
        self.q = {e: [] for e in self.ENGS}
        self.cnt = {}
        self.last_w = {}
        self.readers = {}
        self.waited = {e: {} for e in self.ENGS}
        self.same = same_eng_sync
        self.sem_names = set(self.ENGS)
        self.n_ops = 0

    def _deps(self, eng, reads, writes, own_sem):
        need = {}

        def add(sv):
            if sv is None:
                return
            s, v = sv
            if s == own_sem and (not self.same or eng == "pe"):
                return
            if need.get(s, 0) < v:
                need[s] = v

        for k in reads:
            add(self.last_w.get(k))
        for k in writes:
            add(self.last_w.get(k))
            for s, v in self.readers.get(k, {}).items():
                add((s, v))
        out = []
        for s, v in need.items():
            if self.waited[eng].get(s, 0) < v:
                self.waited[eng][s] = v
                out.append((s, v))
        return out

    def _mark(self, reads, writes, sem, val):
        for k in reads:
            self.readers.setdefault(k, {})[sem] = val
        for k in writes:
            self.last_w[k] = (sem, val)
            self.readers[k] = {}

    def op(self, eng, fn, reads=(), writes=()):
        import os
        if self.n_ops >= int(os.environ.get("PROG_MAXOPS", "100000000")):
            return
        waits = self._deps(eng, reads, writes, eng)
        v = self.cnt.get(eng, 0) + 1
        self.cnt[eng] = v
        self.q[eng].append((waits, fn, eng, 1))
        self._mark(reads, writes, eng, v)
        self.n_ops += 1

    def dma(self, queue, sem, pairs, reads=(), writes=(), **kw):
        import os
        if self.n_ops >= int(os.environ.get("PROG_MAXOPS", "100000000")):
            return
        self.n_ops += 1
        self.sem_names.add(sem)
        waits = self._deps(queue, reads, writes, None)
        base = self.cnt.get(sem, 0)
        for i, (o, i_) in enumerate(pairs):
            def fn(e, o=o, i_=i_):
                return e.dma_start(out=o, in_=i_, **kw)
            self.q[queue].append((waits if i == 0 else [], fn, sem, 16))
        v = base + 16 * len(pairs)
        self.cnt[sem] = v
        self._mark(reads, writes, sem, v)

    def wait_all(self, eng, keys):
        waits = self._deps(eng, keys, (), None)
        import os
        if os.environ.get("PROG_MAXOPS"):
            waits = [(s_, v) for s_, v in self.cnt.items() if v > 0]
        self.q[eng].append((waits, None, None, 0))

    def emit(self):
        nc = self.nc
        import contextlib
        with contextlib.ExitStack() as st:
            sems = {n: st.enter_context(nc.semaphore(self.pfx + "s_" + n)) for n in sorted(self.sem_names)}
            block = st.enter_context(nc.Block())

            def replay(name):
                def run(e):
                    for waits, fn, sem, inc in self.q[name]:
                        for s, v in waits:
                            e.wait_ge(sems[s], v)
                        if fn is not None:
                            ins = fn(e)
                            ins.then_inc(sems[sem], inc)
                return run

            block.sync(replay("sp"))
            block.tensor(replay("pe"))
            block.scalar(replay("act"))
            block.vector(replay("dve"))
            block.gpsimd(replay("pool"))


class Alloc:
    CNT = [0]

    def __init__(self, nc):
        import contextlib
        self.nc = nc
        self.st = contextlib.ExitStack()
        Alloc.CNT[0] += 1
        self.pfx = f"a{Alloc.CNT[0]}_"

    def sb(self, name, shape, dt):
        return self.st.enter_context(self.nc.sbuf_tensor(self.pfx + "sb_" + name, list(shape), dt))

    def ps(self, name, shape, dt):
        return self.st.enter_context(self.nc.psum_tensor(self.pfx + "ps_" + name, list(shape), dt))

    def close(self):
        self.st.close()


FF_GROUPS = [(0, 4), (4, 4), (8, 4), (12, 4), (16, 3), (19, 3)]
UNIT = 1024


def build_ffn(NT):
    assert NT % UNIT == 0 or NT == 512
    unit = min(UNIT, NT)
    n_units = NT // unit
    nc = bass.Bass("TRN2", target_bir_lowering=False)
    x = nc.dram_tensor("x", [NT, D], F32, kind="ExternalInput").ap()
    mT = nc.dram_tensor("mT", [D, NT], BF16, kind="ExternalInput").ap()
    w_out = nc.dram_tensor("w_out", [D, D], F32, kind="ExternalInput").ap()
    w_up = nc.dram_tensor("w_up", [D, 2 * DFF], F32, kind="ExternalInput").ap()
    w_down = nc.dram_tensor("w_down", [DFF, D], F32, kind="ExternalInput").ap()
    g_bc = nc.dram_tensor("g_bc", [128, D], F32, kind="ExternalInput").ap()
    ident = nc.dram_tensor("ident", [128, 128], BF16, kind="ExternalInput").ap()
    y = nc.dram_tensor("y", [NT, D], F32, kind="ExternalOutput").ap()

    A = Alloc(nc)
    P = Prog(nc)
    emit_ffn(nc, A, P, x, mT, w_out, w_up, w_down, g_bc, ident, y, NT, unit, n_units)
    P.wait_all("sp", [("y", u) for u in range(n_units)])
    P.emit()
    A.close()
    return nc


def emit_ffn(nc, A, P, x, mT, w_out, w_up, w_down, g_bc, ident, y, NT, unit, n_units):
    ntile = unit // 512
    nblk = unit // 128
    wout_sb = A.sb("wout_sb", [128, 8, D], BF16)
    stage = [A.sb(f"stage{i}", [128, 2048], F32) for i in range(2)]
    xres = A.sb("xres", [128, nblk, D], F32)
    hnT = A.sb("hnT", [128, 8, unit], BF16)
    mts = [A.sb(f"mts{i}", [128, 8, 512], BF16) for i in range(2)]
    wup = [A.sb(f"wup{i}", [128, 8, 2, 512], BF16) for i in range(2)]
    wdn = [A.sb(f"wdn{i}", [128, 4, D], BF16) for i in range(2)]
    actT = [A.sb(f"actT{i}", [128, 4, 512], BF16) for i in range(2)]
    sg = [A.sb(f"sg{i}", [128, 512], F32) for i in range(2)]
    gbc = A.sb("gbc", [128, D], F32)
    idn = A.sb("idn", [128, 128], BF16)
    hn = [A.sb(f"hn{i}", [128, D], BF16) for i in range(2)]
    sq = A.sb("sq", [128, D], BF16)
    stat = A.sb("stat", [128, 64], F32)
    pb = [A.ps(f"pb{i}", [128, 512], F32) for i in range(7)]
    ptr = A.ps("ptr", [128, 8, 128], BF16)

    st_i = [0]

    def load_cast(dst_ap, src_ap, shape, dkey):
        i = st_i[0] % 2
        st_i[0] += 1
        n = int(np.prod(shape))
        sv = stage[i][:, 0:n]
        if len(shape) == 2:
            sv = sv.rearrange("p (a b) -> p a b", a=shape[0])
        P.dma("sp", f"stg{i}", [(sv, src_ap)], writes=[("stage", i)])
        if i == 0:
            P.op("pool", lambda e: e.tensor_copy(out=dst_ap, in_=sv),
                 reads=[("stage", i)], writes=[dkey])
        else:
            P.op("act", lambda e: e.activation(out=dst_ap, in_=sv, func=AF.Copy),
                 reads=[("stage", i)], writes=[dkey])

    P.dma("sp", "const", [(gbc[:], g_bc), (idn[:], ident)], writes=["gbc", "idn"])
    for c2 in range(4):
        load_cast(wout_sb[:, 2 * c2:2 * c2 + 2, :],
                  w_out[c2 * 256:(c2 + 1) * 256, :].rearrange("(c p) n -> p c n", p=128),
                  (2, D), ("wout", c2))

    pbi = [0]

    def next_pb(lo, hi):
        i = lo + pbi[0] % (hi - lo)
        pbi[0] += 1
        return i

    for u in range(n_units):
        t0 = u * unit
        for t in range(ntile):
            tt = t0 + t * 512
            mi = (u * ntile + t) % 2
            P.dma("sp", f"mts{mi}", [(mts[mi][:], mT[:, tt:tt + 512].rearrange("(c p) t -> p c t", p=128))],
                  writes=[("mts", mi)])
            xv = xres[:, 4 * t:4 * t + 4, :]
            P.dma("sp", f"xin{t % 2}", [(xv, x[tt:tt + 512, :].rearrange("(b p) d -> p b d", p=128))],
                  writes=[("xres", 4 * t + b) for b in range(4)])
            for b in range(4):
                bb = 4 * t + b
                for h in range(2):
                    k = next_pb(0, 4)
                    for c in range(8):
                        P.op("pe", lambda e, k=k, c=c, b=b, h=h, mi=mi: e.matmul(
                            pb[k][:], lhsT=mts[mi][:, c, b * 128:(b + 1) * 128],
                            rhs=wout_sb[:, c, h * 512:(h + 1) * 512], start=(c == 0), stop=(c == 7)),
                            reads=[("mts", mi), ("wout", c // 2)], writes=[("pb", k)])
                    P.op("dve", lambda e, k=k, bb=bb, h=h: e.tensor_tensor(
                        out=xres[:, bb, h * 512:(h + 1) * 512], in0=pb[k][:],
                        in1=xres[:, bb, h * 512:(h + 1) * 512], op=ALU.add),
                        reads=[("pb", k)], writes=[("xres", bb)])
                P.op("act", lambda e, bb=bb: e.activation(out=sq[:], in_=xres[:, bb, :], func=AF.Square,
                                                          accum_out=stat[:, bb:bb + 1]),
                     reads=[("xres", bb)], writes=["sq", ("stat", bb)])
                P.op("act", lambda e, bb=bb: e.activation(out=stat[:, 16 + bb:17 + bb], in_=stat[:, bb:bb + 1],
                                                          func=AF.Ln, scale=1.0 / D, bias=EPS),
                     reads=[("stat", bb)], writes=[("stat2", bb)])
                P.op("act", lambda e, bb=bb: e.activation(out=stat[:, 32 + bb:33 + bb], in_=stat[:, 16 + bb:17 + bb],
                                                          func=AF.Exp, scale=-0.5),
                     reads=[("stat2", bb)], writes=[("stat3", bb)])
                hi = bb % 2
                P.op("dve", lambda e, bb=bb, hi=hi: e.scalar_tensor_tensor(
                    out=hn[hi][:], in0=xres[:, bb, :], scalar=stat[:, 32 + bb:33 + bb], in1=gbc[:],
                    op0=ALU.mult, op1=ALU.mult),
                    reads=[("xres", bb), ("stat3", bb), "gbc"], writes=[("hn", hi)])
                for c in range(8):
                    P.op("pe", lambda e, c=c, hi=hi: e.transpose(ptr[:, c, :], hn[hi][:, c * 128:(c + 1) * 128], idn[:]),
                         reads=[("hn", hi), "idn"], writes=["ptr"])
                P.op("act", lambda e, bb=bb: e.activation(out=hnT[:, :, bb * 128:(bb + 1) * 128], in_=ptr[:],
                                                          func=AF.Copy),
                     reads=["ptr"], writes=[("hnT", bb)])
        for gi, (j0, G) in enumerate(FF_GROUPS):
            wi = (u * len(FF_GROUPS) + gi) % 2
            gw = G * 128
            for s in range(2):
                for ch in range(2):
                    c0 = s * DFF + j0 * 128
                    load_cast(wup[wi][:, 4 * ch:4 * ch + 4, s, 0:gw],
                              w_up[ch * 512:(ch + 1) * 512, c0:c0 + gw].rearrange("(c p) n -> p c n", p=128),
                              (4, gw), ("wup", wi, s, ch))
            for jh in range(0, G, 2):
                n = min(2, G - jh)
                load_cast(wdn[wi][:, jh:jh + n, :],
                          w_down[(j0 + jh) * 128:(j0 + jh + n) * 128, :].rearrange("(j p) n -> p j n", p=128),
                          (n, D), ("wdn", wi, jh // 2))
            for t in range(ntile):
                ai = (gi * ntile + t) % 2
                for j in range(G):
                    kg = next_pb(0, 4)
                    ku = next_pb(0, 4)
                    for s, k in ((0, kg), (1, ku)):
                        for c in range(8):
                            P.op("pe", lambda e, k=k, c=c, s=s, j=j, t=t, wi=wi: e.matmul(
                                pb[k][:], lhsT=wup[wi][:, c, s, j * 128:(j + 1) * 128],
                                rhs=hnT[:, c, t * 512:(t + 1) * 512], start=(c == 0), stop=(c == 7)),
                                reads=[("wup", wi, s, c // 4)] + [("hnT", 4 * t + b) for b in range(4)],
                                writes=[("pb", k)])
                    si = j % 2
                    P.op("act", lambda e, kg=kg, si=si: e.activation(out=sg[si][:], in_=pb[kg][:], func=AF.Silu),
                         reads=[("pb", kg)], writes=[("sg", si)])
                    P.op("dve", lambda e, ku=ku, si=si, ai=ai, j=j: e.tensor_tensor(
                        out=actT[ai][:, j, :], in0=pb[ku][:], in1=sg[si][:], op=ALU.mult),
                        reads=[("pb", ku), ("sg", si)], writes=[("actT", ai, j)])
                for b in range(4):
                    bb = 4 * t + b
                    for h in range(2):
                        k = 4 + next_pb(0, 3)
                        for j in range(G):
                            P.op("pe", lambda e, k=k, j=j, b=b, h=h, ai=ai, wi=wi, G=G: e.matmul(
                                pb[k][:], lhsT=actT[ai][:, j, b * 128:(b + 1) * 128],
                                rhs=wdn[wi][:, j, h * 512:(h + 1) * 512], start=(j == 0), stop=(j == G - 1)),
                                reads=[("actT", ai, j), ("wdn", wi, j // 2)], writes=[("pb", k)])
                        P.op("dve", lambda e, k=k, bb=bb, h=h: e.tensor_tensor(
                            out=xres[:, bb, h * 512:(h + 1) * 512], in0=pb[k][:],
                            in1=xres[:, bb, h * 512:(h + 1) * 512], op=ALU.add),
                            reads=[("pb", k)], writes=[("xres", bb)])
        P.dma("sp", "yout", [(y[t0:t0 + unit, :].rearrange("(b p) d -> p b d", p=128), xres[:])],
              reads=[("xres", bb) for bb in range(nblk)], writes=[("y", u)])


FM = [("gq", 128), ("gk", 128), ("glr", 16), ("gg0", 96), ("gg1", 96),
      ("sq0", 64), ("sq1", 64), ("sq2", 64), ("sk0", 64), ("sk1", 64), ("sk2", 64),
      ("hq0", 128), ("hq1", 128), ("hf0", 128), ("hf1", 128), ("hgt0", 64), ("hgt1", 64)]
FM_OFF = {}
_o = 0
for _n, _m in FM:
    FM_OFF[_n] = (_o, _m)
    _o += ((_m + 63) // 64) * 64
TM_OFF = _o
WC = _o + 512
CV_BDEC, CV_GLAG, CV_SBQ, CV_SBK, CV_SBO, CV_HGO, CV_LB = 0, 1, 2, 3, 4, 5, 6
CF_R128, CF_R64, CF_TRI, CF_BD64, CF_STRICT = 0, 512, 1024, 1152, 1280
CF_M0, CF_M1, CF_P0, CF_P1 = 1408, 1920, 2432, 2433
CF_W = 2436
CB_NEGINCL, CB_NEGONES, CB_ONES, CB_ZERO, CB_IDENT = 0, 128, 256, 384, 512
CB_W = 640


def make_consts():
    f = np.zeros((128, CF_W), np.float32)
    r = np.ones(512, np.float32); r[::128] = 0; f[:, CF_R128:CF_R128 + 512] = r
    r = np.ones(512, np.float32); r[::64] = 0; f[:, CF_R64:CF_R64 + 512] = r
    j = np.arange(128)[:, None]; i = np.arange(128)[None, :]
    f[:, CF_TRI:CF_TRI + 128] = (j <= i)
    f[:, CF_BD64:CF_BD64 + 128] = (j <= i) & ((j // 64) == (i // 64))
    f[:, CF_STRICT:CF_STRICT + 128] = (j < i)
    cc = np.arange(512)
    f[:, CF_M0:CF_M0 + 512] = ((cc % 128) < 64)
    f[:, CF_M1:CF_M1 + 512] = ((cc % 128) >= 64)
    f[0:64, CF_P0] = 1
    f[64:128, CF_P1] = 1
    b = np.zeros((128, CB_W), np.float32)
    b[:, CB_NEGINCL:CB_NEGINCL + 128] = -(j >= i).astype(np.float32)
    b[:, CB_NEGONES:CB_NEGONES + 128] = -1
    b[:, CB_ONES:CB_ONES + 128] = 1
    b[:, CB_IDENT:CB_IDENT + 128] = np.eye(128)
    return f, b.astype(NPBF)


def build_mixer(T, li, parts="sgh"):
    nc = bass.Bass("TRN2", target_bir_lowering=False)
    dr = {}
    dr["x"] = nc.dram_tensor("x", [T, D], F32, kind="ExternalInput").ap()
    dr["w_in"] = nc.dram_tensor("w_in", [D, WC], F32, kind="ExternalInput").ap()
    dr["g_bc"] = nc.dram_tensor("g_bc", [128, D], F32, kind="ExternalInput").ap()
    dr["cvec"] = nc.dram_tensor("cvec", [128, 16], F32, kind="ExternalInput").ap()
    dr["wdec"] = nc.dram_tensor("wdec", [16, 128], F32, kind="ExternalInput").ap()
    dr["cf"] = nc.dram_tensor("cf", [128, CF_W], F32, kind="ExternalInput").ap()
    dr["cb"] = nc.dram_tensor("cb", [128, CB_W], BF16, kind="ExternalInput").ap()
    dr["mT"] = nc.dram_tensor("mT", [512, T], BF16, kind="ExternalOutput").ap()
    A = Alloc(nc)
    P = Prog(nc)
    emit_mixer(nc, A, P, dr, T, li, parts)
    P.wait_all("sp", [("mT", t) for t in range(T // 512)])
    P.emit()
    A.close()
    return nc


def emit_mixer(nc, A, P, dr, T, li, parts):
    NTILE = T // 512
    NBLK = T // 128
    win = A.sb("win", [128, 8, WC], BF16)
    stage = [A.sb(f"stage{i}", [128, 1024], F32) for i in range(2)]
    gbc = A.sb("gbc", [128, D], F32)
    cv = A.sb("cv", [128, 16], F32)
    cv2 = A.sb("cv2", [128, 16], F32)
    wdec_f = A.sb("wdec_f", [16, 128], F32)
    wdec = A.sb("wdec", [16, 128], BF16)
    cf = A.sb("cf", [128, CF_W], F32)
    cb = A.sb("cb", [128, CB_W], BF16)
    xt = A.sb("xt", [128, 4, D], F32)
    sq = A.sb("sq", [128, D], BF16)
    stat = A.sb("stat", [128, 16], F32)
    hn = [A.sb(f"hn{i}", [128, D], BF16) for i in range(2)]
    hT = A.sb("hT", [128, 8, 512], BF16)
    KT = A.sb("KT", [64, 3, T], BF16)
    Vall = A.sb("Vall", [128, NBLK, 192], BF16)
    vt = A.sb("vt", [128, 4, 320], BF16)
    qT = A.sb("qT", [64, 3, 512], BF16)
    rawf = [A.sb(f"rawf{i}", [128, 512], F32) for i in range(2)]
    sqb = A.sb("sqb", [128, 512], BF16)
    lnv = A.sb("lnv", [128, 512], F32)
    rstd = A.sb("rstd", [128, 512], F32)
    ef = [A.sb(f"ef{i}", [128, 512], F32) for i in range(2)]
    spb = [A.sb(f"spb{i}", [128, 512], BF16) for i in range(2)]
    Sb = A.sb("Sb", [128, 512], BF16)
    wg = [A.sb(f"wg{i}", [128, 512], BF16) for i in range(2)]
    sgl = [A.sb(f"sgl{i}", [96, 512], F32) for i in range(2)]
    shg = [A.sb(f"shg{i}", [64, 512], F32) for i in range(2)]
    qg = A.sb("qg", [128, 512], F32)
    kg = A.sb("kg", [128, 512], F32)
    glr = A.sb("glr", [16, 512], BF16)
    qh = [A.sb(f"qh{i}", [128, 512], F32) for i in range(2)]
    kh = [A.sb(f"kh{i}", [128, 512], F32) for i in range(2)]
    lf = A.sb("lf", [128, 512], F32)
    hfa = A.sb("hfa", [128, 512], F32)
    hfb = A.sb("hfb", [128, 512], F32)
    Gs = A.sb("Gs", [128, 512], F32)
    dlt = A.sb("dlt", [128, 512], F32)
    E1 = A.sb("E1", [128, 512], F32)
    E2 = A.sb("E2", [128, 512], F32)
    dl = A.sb("dl", [128, 8], F32)
    qd = A.sb("qd", [128, 512], BF16)
    kd = A.sb("kd", [128, 512], BF16)
    ATs = [A.sb(f"ATs{i}", [128, 128], BF16) for i in range(2)]
    qz = [A.sb(f"qz{i}", [128, 512], BF16) for i in range(2)]
    vz = [A.sb(f"vz{i}", [128, 64], BF16) for i in range(2)]
    ktok = [A.sb(f"ktok{i}", [128, 128], BF16) for i in range(2)]
    Sg = A.sb("Sg", [128, 96], F32)
    Sgb = A.sb("Sgb", [128, 96], BF16)
    Sh = [A.sb(f"Sh{i}", [128, 64], F32) for i in range(2)]
    Shb = [A.sb(f"Shb{i}", [128, 64], BF16) for i in range(2)]
    onrm = A.sb("onrm", [128, 512], F32)
    mo = [A.sb("mo0", [128, 7, 512], BF16)] * 2
    pb = [A.ps(f"pb{i}", [128, 512], F32) for i in range(7)]
    ptr = A.ps("ptr", [128, 8, 128], BF16)

    def ones(m):
        return cb[0:m, CB_ONES:CB_ONES + m]

    idn_t = A.sb("idn_t", [128, 128], BF16)
    idn = idn_t[:]

    st_i = [0]

    def load_cast(dst_ap, src_ap, shape, dkey):
        i = st_i[0] % 2
        st_i[0] += 1
        n = int(np.prod(shape))
        sv = stage[i][:, 0:n]
        if len(shape) == 2:
            sv = sv.rearrange("p (a b) -> p a b", a=shape[0])
        P.dma("sp", f"stg{i}", [(sv, src_ap)], writes=[("stage", i)])
        if i == 0:
            P.op("pool", lambda e: e.tensor_copy(out=dst_ap, in_=sv), reads=[("stage", i)], writes=[dkey])
        else:
            P.op("act", lambda e: e.activation(out=dst_ap, in_=sv, func=AF.Copy), reads=[("stage", i)], writes=[dkey])

    P.dma("sp", "const", [(gbc[:], dr["g_bc"]), (cv[:], dr["cvec"]), (wdec_f[:], dr["wdec"]),
                          (cf[:], dr["cf"]), (cb[:], dr["cb"]), (idn_t[:], dr["cb"][:, CB_IDENT:CB_IDENT + 128])],
          writes=["gbc", "cv", "wdec_f", "cf", "cb"])
    for c in range(8):
        for hf in range(3):
            load_cast(win[:, c, hf * 704:(hf + 1) * 704], dr["w_in"][c * 128:(c + 1) * 128, hf * 704:(hf + 1) * 704],
                      (704,), ("win", c))
    P.op("dve", lambda e: e.tensor_copy(out=wdec[:], in_=wdec_f[:]), reads=["wdec_f"], writes=["wdec"])
    P.op("dve", lambda e: e.tensor_scalar(out=cv2[:, 0:1], in0=cv[:, CV_BDEC:CV_BDEC + 1], scalar1=-1.0, scalar2=None,
                                          op0=ALU.mult), reads=["cv"], writes=["cv2a"])
    P.op("dve", lambda e: e.tensor_scalar(out=cv2[:, 1:2], in0=cv[:, CV_SBQ:CV_SBQ + 1], scalar1=0.125, scalar2=None,
                                          op0=ALU.mult), reads=["cv"], writes=["cv2b"])
    if li == 0:
        P.op("dve", lambda e: e.memset(cv2[:, 2:4], 0.0), writes=["cv2c"])
        P.op("dve", lambda e: e.memset(cv2[:, 4:6], 1.0), writes=["cv2d"])
    else:
        P.op("dve", lambda e: e.tensor_tensor(out=cv2[:, 6:8], in0=cv[:, CV_LB:CV_LB + 2], in1=cv[:, CV_LB + 2:CV_LB + 4],
                                              op=ALU.subtract), reads=["cv"], writes=["cv2e"])
        P.op("act", lambda e: e.activation(out=cv2[:, 8:10], in_=cv2[:, 6:8], func=AF.Exp), reads=["cv2e"], writes=["cv2f"])
        P.op("act", lambda e: e.activation(out=cv2[:, 10:12], in_=cv2[:, 6:8], func=AF.Exp, scale=-1.0),
             reads=["cv2e"], writes=["cv2g"])
        P.op("dve", lambda e: e.tensor_scalar(out=cv2[:, 8:12], in0=cv2[:, 8:12], scalar1=1.0, scalar2=None, op0=ALU.add),
             reads=["cv2f", "cv2g"], writes=["cv2h"])
        P.op("dve", lambda e: e.reciprocal(out=cv2[:, 2:4], in_=cv2[:, 8:10]), reads=["cv2h"], writes=["cv2c"])
        P.op("dve", lambda e: e.reciprocal(out=cv2[:, 4:6], in_=cv2[:, 10:12]), reads=["cv2h"], writes=["cv2d"])
    P.op("dve", lambda e: e.memset(mo[0][:], 0.0), writes=[("mo", 0, j) for j in range(7)])
    P.op("dve", lambda e: e.memset(Sg[:], 0.0), writes=["Sg"])
    for h in range(2):
        P.op("dve", lambda e, h=h: e.memset(Sh[h][:], 0.0), writes=[("Sh", h)])

    rr = {"g": 0}

    def bank(kind):
        if kind == "w":
            return 3
        k = rr["g"] % 3
        rr["g"] += 1
        return k

    def headnorm(src, srckeys, M, gain, dst, dstkeys, extra=None, extrakeys=()):
        P.op("act", lambda e: e.activation(out=sqb[0:M, :], in_=src, func=AF.Square), reads=srckeys, writes=["sqb"])
        k = bank("g")
        P.op("pe", lambda e: e.matmul(pb[k][0:M, :], lhsT=ones(M), rhs=sqb[0:M, :], start=True, stop=True),
             reads=["sqb", "cb"], writes=[("pb", k)])
        P.op("act", lambda e: e.activation(out=lnv[0:M, :], in_=pb[k][0:M, :], func=AF.Ln, scale=1.0 / M, bias=EPS),
             reads=[("pb", k)], writes=["lnv"])
        P.op("act", lambda e: e.activation(out=rstd[0:M, :], in_=lnv[0:M, :], func=AF.Exp, scale=-0.5),
             reads=["lnv"], writes=["rstd"])
        if extra is None:
            P.op("dve", lambda e: e.scalar_tensor_tensor(out=dst, in0=src, scalar=gain, in1=rstd[0:M, :],
                                                         op0=ALU.mult, op1=ALU.mult),
                 reads=list(srckeys) + ["rstd", "cv", "cv2b"], writes=dstkeys)
        else:
            P.op("dve", lambda e: e.scalar_tensor_tensor(out=onrm[0:M, :], in0=src, scalar=gain, in1=rstd[0:M, :],
                                                         op0=ALU.mult, op1=ALU.mult),
                 reads=list(srckeys) + ["rstd", "cv"], writes=["onrm"])
            P.op("dve", lambda e: e.tensor_tensor(out=dst, in0=onrm[0:M, :], in1=extra, op=ALU.mult),
                 reads=["onrm"] + list(extrakeys), writes=dstkeys)

    def proj_fm(name):
        off, M = FM_OFF[name]
        k = bank("g")
        for c in range(8):
            P.op("pe", lambda e, c=c: e.matmul(pb[k][0:M, :], lhsT=win[:, c, off:off + M], rhs=hT[:, c, :],
                                               start=(c == 0), stop=(c == 7)),
                 reads=[("win", c), "hT"], writes=[("pb", k)])
        return k, M

    for ti in range(NTILE):
        tt = ti * 512
        mi = 0
        if "hT_in" in dr:
            P.dma("sp", "hTin", [(hT[:], dr["hT_in"][ti].rearrange("p (c t) -> p c t", c=8))], writes=["hT"])
        else:
            P.dma("sp", "xin", [(xt[:], dr["x"][tt:tt + 512, :].rearrange("(b p) d -> p b d", p=128))], writes=["xt"])
        for b in range(4 if "hT_in" not in dr else 0):
            P.op("act", lambda e, b=b: e.activation(out=sq[:], in_=xt[:, b, :], func=AF.Square, accum_out=stat[:, b:b + 1]),
                 reads=["xt"], writes=["sq", ("stat", b)])
            P.op("act", lambda e, b=b: e.activation(out=stat[:, 4 + b:5 + b], in_=stat[:, b:b + 1], func=AF.Ln,
                                                    scale=1.0 / D, bias=EPS), reads=[("stat", b)], writes=[("stat2", b)])
            P.op("act", lambda e, b=b: e.activation(out=stat[:, 8 + b:9 + b], in_=stat[:, 4 + b:5 + b], func=AF.Exp,
                                                    scale=-0.5), reads=[("stat2", b)], writes=[("stat3", b)])
            hi = b % 2
            P.op("dve", lambda e, b=b, hi=hi: e.scalar_tensor_tensor(out=hn[hi][:], in0=xt[:, b, :],
                                                                     scalar=stat[:, 8 + b:9 + b], in1=gbc[:],
                                                                     op0=ALU.mult, op1=ALU.mult),
                 reads=["xt", ("stat3", b), "gbc"], writes=[("hn", hi)])
            for c in range(8):
                P.op("pe", lambda e, c=c, hi=hi: e.transpose(ptr[:, c, :], hn[hi][:, c * 128:(c + 1) * 128], idn),
                     reads=[("hn", hi), "cb"], writes=["ptr"])
            P.op("act", lambda e, b=b: e.activation(out=hT[:, :, b * 128:(b + 1) * 128], in_=ptr[:], func=AF.Copy),
                 reads=["ptr"], writes=["hT"])
        if "hT_out" in dr:
            P.dma("pool", "hTout", [(dr["hT_out"][ti].rearrange("p (c t) -> p c t", c=8), hT[:])], reads=["hT"],
                  writes=[("hTs", ti)])
        for b in range(4):
            k = bank("g")
            for c in range(8):
                P.op("pe", lambda e, c=c, b=b, k=k: e.matmul(pb[k][:], lhsT=hT[:, c, b * 128:(b + 1) * 128],
                                                             rhs=win[:, c, TM_OFF:TM_OFF + 512], start=(c == 0), stop=(c == 7)),
                     reads=[("win", c), "hT"], writes=[("pb", k)])
            P.op("act", lambda e, b=b, k=k, ti=ti: e.activation(out=Vall[:, ti * 4 + b, :], in_=pb[k][:, 0:192], func=AF.Copy),
                 reads=[("pb", k)], writes=[("Vall", ti * 4 + b)])
            P.op("act", lambda e, b=b, k=k: e.activation(out=vt[:, b, :], in_=pb[k][:, 192:512], func=AF.Copy),
                 reads=[("pb", k)], writes=[("vt", b)])
        for h in range(2):
            k, M = proj_fm(f"gg{h}")
            P.op("act", lambda e, h=h, k=k: e.activation(out=sgl[h][:], in_=pb[k][0:96, :], func=AF.Silu),
                 reads=[("pb", k)], writes=[("sgl", h)])
        for h in range(2):
            k, M = proj_fm(f"hgt{h}")
            P.op("act", lambda e, h=h, k=k: e.activation(out=shg[h][:], in_=pb[k][0:64, :], func=AF.Silu),
                 reads=[("pb", k)], writes=[("shg", h)])

        sb_steps = []
        if "s" in parts:
            for h in range(3):
                k, M = proj_fm(f"sk{h}")
                ri = h % 2
                P.op("act", lambda e, k=k, ri=ri: e.activation(out=rawf[ri][0:64, :], in_=pb[k][0:64, :], func=AF.Copy),
                     reads=[("pb", k)], writes=[("rawf", ri)])
                headnorm(rawf[ri][0:64, :], [("rawf", ri)], 64, cv[0:64, CV_SBK:CV_SBK + 1],
                         KT[:, h, tt:tt + 512], [("KT", h, ti)])
                k, M = proj_fm(f"sq{h}")
                ri = (h + 1) % 2
                P.op("act", lambda e, k=k, ri=ri: e.activation(out=rawf[ri][0:64, :], in_=pb[k][0:64, :], func=AF.Copy),
                     reads=[("pb", k)], writes=[("rawf", ri)])
                headnorm(rawf[ri][0:64, :], [("rawf", ri)], 64, cv2[0:64, 1:2], qT[:, h, :], [("qT", h)])
            nkb = 4 * ti + 4
            pairs = [(h, idx, kb) for h in range(3) for idx, kb in enumerate(range(nkb - 1, -1, -1))]
            info = {}

            def stageA(n):
                h, idx, kb = pairs[n]
                r = kb - 4 * ti
                c0 = 128 * r if r >= 0 else 0
                ei = n % 2
                ktile = kb // 4
                kz = bank("z")
                info[n] = (c0, ei, r)
                P.op("pe", lambda e: e.matmul(pb[kz][:, c0:512], lhsT=KT[:, h, kb * 128:(kb + 1) * 128], rhs=qT[:, h, c0:512],
                                              start=True, stop=True),
                     reads=[("KT", h, ktile), ("qT", h)], writes=[("pb", kz)])
                P.op("act", lambda e: e.activation(out=ef[ei][:, c0:512], in_=pb[kz][:, c0:512], func=AF.Exp),
                     reads=[("pb", kz)], writes=[("ef", ei)])
                P.op("act", lambda e: e.activation(out=spb[ei][:, c0:512], in_=ef[ei][:, c0:512], func=AF.Ln, bias=1.0),
                     reads=[("ef", ei)], writes=[("spb", ei)])
                if r >= 0:
                    P.op("dve", lambda e: e.tensor_tensor(out=spb[ei][:, c0:c0 + 128], in0=spb[ei][:, c0:c0 + 128],
                                                          in1=cf[:, CF_STRICT:CF_STRICT + 128], op=ALU.mult),
                         reads=[("spb", ei), "cf"], writes=[("spb", ei)])

            def stageB1(n):
                h, idx, kb = pairs[n]
                c0, ei, r = info[n]
                first = (idx == 0)
                last = (kb == 0)
                ktile = kb // 4
                kw = bank("w")
                P.op("pe", lambda e: e.matmul(pb[kw][:, c0:512], lhsT=KT[:, h, kb * 128:(kb + 1) * 128], rhs=qT[:, h, c0:512],
                                              start=True, stop=False),
                     reads=[("KT", h, ktile), ("qT", h)], writes=[("pb", kw)])
                P.op("pe", lambda e: e.matmul(pb[kw][:, c0:512], lhsT=cb[:, CB_NEGINCL:CB_NEGINCL + 128], rhs=spb[ei][:, c0:512],
                                              start=False, stop=first),
                     reads=[("spb", ei), "cb"], writes=[("pb", kw)])
                if not first:
                    P.op("pe", lambda e: e.matmul(pb[kw][:, c0:512], lhsT=cb[:, CB_NEGONES:CB_NEGONES + 128], rhs=Sb[:, c0:512],
                                                  start=False, stop=True),
                         reads=["Sb", "cb"], writes=[("pb", kw)])
                P.op("act", lambda e: e.activation(out=wg[ei][:, c0:512], in_=pb[kw][:, c0:512], func=AF.Exp),
                     reads=[("pb", kw)], writes=[("wg", ei)])
                if r >= 0:
                    P.op("dve", lambda e: e.tensor_tensor(out=wg[ei][:, c0:c0 + 128], in0=wg[ei][:, c0:c0 + 128],
                                                          in1=cf[:, CF_STRICT:CF_STRICT + 128], op=ALU.mult),
                         reads=[("wg", ei), "cf"], writes=[("wg", ei)])
                if not last:
                    if first:
                        P.op("dve", lambda e: e.memset(Sb[:], 0.0), writes=["Sb"])
                    P.op("dve", lambda e: e.tensor_tensor(out=Sb[:, c0:512], in0=Sb[:, c0:512], in1=spb[ei][:, c0:512], op=ALU.add),
                         reads=[("spb", ei), "Sb"], writes=["Sb"])

            def stageB2(n):
                h, idx, kb = pairs[n]
                c0, ei, r = info[n]
                first = (idx == 0)
                last = (kb == 0)
                if first:
                    P.op("pe", lambda e: e.matmul(pb[6][0:64, :], lhsT=cb[:, CB_ZERO:CB_ZERO + 64], rhs=cb[:, 0:512],
                                                  start=True, stop=False), reads=["cb"], writes=[("pb", 6)])
                P.op("pe", lambda e: e.matmul(pb[6][0:64, c0:512], lhsT=Vall[:, kb, h * 64:(h + 1) * 64], rhs=wg[ei][:, c0:512],
                                              start=False, stop=last),
                     reads=[("wg", ei), ("Vall", kb)], writes=[("pb", 6)])
                if last:
                    headnorm(pb[6][0:64, :], [("pb", 6)], 64, cv[0:64, CV_SBO:CV_SBO + 1], mo[mi][0:64, 2 + h, :],
                             [("mo", mi, 2 + h)])

            npairs = len(pairs)
            sb_steps.append(lambda: stageA(0))
            for n in range(npairs):
                if n + 1 < npairs:
                    sb_steps.append(lambda n=n: stageA(n + 1))
                sb_steps.append(lambda n=n: stageB1(n))
                if n >= 1:
                    sb_steps.append(lambda n=n: stageB2(n - 1))
            sb_steps.append(lambda: stageB2(npairs - 1))

        def hg_gen():
            for h in range(2):
                k, M = proj_fm(f"hq{h}")
                P.op("act", lambda e, k=k, h=h: e.activation(out=qh[h][:], in_=pb[k][:], func=AF.Copy),
                     reads=[("pb", k)], writes=[("qh", h)])
                k, M = proj_fm(f"hf{h}")
                P.op("act", lambda e, k=k: e.activation(out=hfa[:], in_=pb[k][:], func=AF.Exp, scale=-1.0),
                     reads=[("pb", k)], writes=["hfa"])
                yield
                P.op("dve", lambda e: e.tensor_scalar(out=hfa[:], in0=hfa[:], scalar1=1.0, scalar2=None, op0=ALU.add),
                     reads=["hfa"], writes=["hfa"])
                P.op("dve", lambda e: e.reciprocal(out=hfb[:], in_=hfa[:]), reads=["hfa"], writes=["hfb"])
                P.op("act", lambda e, h=h: e.activation(out=hfb[:], in_=hfb[:], func=AF.Identity, scale=cv2[:, 4 + h:5 + h],
                                                        bias=cv2[:, 2 + h:3 + h]),
                     reads=["hfb", "cv2c", "cv2d"], writes=["hfb"])
                P.op("act", lambda e: e.activation(out=lf[:], in_=hfb[:], func=AF.Ln), reads=["hfb"], writes=["lf"])
                P.op("act", lambda e, h=h: e.activation(out=kh[h][:], in_=hfb[:], func=AF.Identity, scale=-1.0, bias=1.0),
                     reads=["hfb"], writes=[("kh", h)])
                yield
                yield from lin_attn(P, L_, "hg", ti, mi, h)

        def gla_gen():
            k, M = proj_fm("gq")
            P.op("act", lambda e, k=k: e.activation(out=qg[:], in_=pb[k][:], func=AF.Copy, scale=48.0 ** -0.5),
                 reads=[("pb", k)], writes=["qg"])
            k, M = proj_fm("gk")
            P.op("act", lambda e, k=k: e.activation(out=kg[:], in_=pb[k][:], func=AF.Copy), reads=[("pb", k)], writes=["kg"])
            yield
            k, M = proj_fm("glr")
            P.op("act", lambda e, k=k: e.activation(out=glr[:], in_=pb[k][0:16, :], func=AF.Copy), reads=[("pb", k)], writes=["glr"])
            k = bank("g")
            P.op("pe", lambda e, k=k: e.matmul(pb[k][:], lhsT=wdec[:], rhs=glr[:], start=True, stop=True),
                 reads=["wdec", "glr"], writes=[("pb", k)])
            P.op("act", lambda e, k=k: e.activation(out=hfa[:], in_=pb[k][:], func=AF.Exp, scale=-1.0, bias=cv2[:, 0:1]),
                 reads=[("pb", k), "cv2a"], writes=["hfa"])
            P.op("act", lambda e: e.activation(out=lf[:], in_=hfa[:], func=AF.Ln, bias=1.0), reads=["hfa"], writes=["lf"])
            yield
            yield from lin_attn(P, L_, "gla", ti, mi)

        def bg_gen():
            if "h" in parts:
                yield from hg_gen()
            if "g" in parts:
                yield from gla_gen()

        L_ = dict(locals())
        bg = bg_gen()
        n_bg = 2 * 11 + 8
        every = max(1, len(sb_steps) // n_bg) if sb_steps else 1
        for i, st in enumerate(sb_steps):
            st()
            if i % every == every - 1:
                next(bg, None)
        for _ in bg:
            pass

        outs = []
        rows = [(0, 96), (96, 96), (192, 64), (256, 64), (320, 64), (384, 64), (448, 64)]
        for j, (r0, m) in enumerate(rows):
            outs.append((dr["mT"][r0:r0 + m, tt:tt + 512], mo[mi][0:m, j, :]))
        P.dma("pool", f"mout{mi}", outs, reads=[("mo", mi, j) for j in range(7)], writes=[("mT", ti)])


def lin_attn(P, L, kind, ti, mi, h=0):
    cf, cb, pb, ptr = L["cf"], L["cb"], L["pb"], L["ptr"]
    Gs, dlt, E1, E2, dl, qd, kd = L["Gs"], L["dlt"], L["E1"], L["E2"], L["dl"], L["qd"], L["kd"]
    ATs, ktok, vt, lf = L["ATs"], L["ktok"], L["vt"], L["lf"]
    bank, headnorm, mo, cv = L["bank"], L["headnorm"], L["mo"], L["cv"]
    idn = L["idn"]
    if kind == "gla":
        C, nb, s1, s2 = 128, 4, 1.0 / 16, -1.0 / 16
        q, k, qkeys, kkeys = L["qg"], L["kg"], ["qg"], ["kg"]
        roff, mask = CF_R128, CF_TRI
    else:
        C, nb, s1, s2 = 64, 8, -1.0, 1.0
        q, k, qkeys, kkeys = L["qh"][h], L["kh"][h], [("qh", h)], [("kh", h)]
        roff, mask = CF_R64, CF_BD64
    P.op("dve", lambda e: e.tensor_tensor_scan(out=Gs[:], data0=cf[:, roff:roff + 512], data1=lf[:], initial=0.0,
                                               op0=ALU.mult, op1=ALU.add), reads=["lf", "cf"], writes=["Gs"])
    gv = Gs[:].rearrange("p (b c) -> p b c", c=C)
    P.op("dve", lambda e: e.tensor_tensor(out=dlt[:].rearrange("p (b c) -> p b c", c=C),
                                          in0=gv[:, :, C - 1:C].to_broadcast([128, nb, C]), in1=gv, op=ALU.subtract),
         reads=["Gs"], writes=["dlt"])
    P.op("act", lambda e: e.activation(out=E1[:], in_=dlt[:], func=AF.Exp, scale=s1), reads=["dlt"], writes=["E1"])
    P.op("act", lambda e: e.activation(out=E2[:], in_=dlt[:], func=AF.Exp, scale=-s1), reads=["dlt"], writes=["E2"])
    P.op("act", lambda e: e.activation(out=dl[:, 0:nb], in_=gv[:, :, C - 1], func=AF.Exp, scale=s2), reads=["Gs"], writes=["dl"])
    P.op("dve", lambda e: e.tensor_tensor(out=qd[:], in0=q[:], in1=E1[:], op=ALU.mult), reads=qkeys + ["E1"], writes=["qd"])
    P.op("dve", lambda e: e.tensor_tensor(out=kd[:], in0=k[:], in1=E2[:], op=ALU.mult), reads=kkeys + ["E2"], writes=["kd"])
    if kind != "gla":
        for s in range(2):
            P.op("dve", lambda e, s=s: e.tensor_tensor(out=L["qz"][s][:], in0=qd[:], in1=cf[:, CF_M0 + 512 * s:CF_M0 + 512 * (s + 1)], op=ALU.mult),
                 reads=["qd", "cf"], writes=[("qz", s)])
    yield
    for b4 in range(4):
        cs = slice(b4 * 128, (b4 + 1) * 128)
        ai = b4 % 2
        P.op("pe", lambda e, cs=cs: e.transpose(ptr[:, 0, :], kd[:, cs], idn), reads=["kd", "cb"], writes=["ptr"])
        P.op("act", lambda e, ai=ai: e.activation(out=ktok[ai][:], in_=ptr[:, 0, :], func=AF.Copy), reads=["ptr"], writes=[("ktok", ai)])
        if kind == "gla":
            Sg, Sgb = L["Sg"], L["Sgb"]
            P.op("dve", lambda e, b4=b4: e.tensor_scalar(out=Sgb[:], in0=Sg[:], scalar1=dl[:, b4:b4 + 1], scalar2=None, op0=ALU.mult),
                 reads=["Sg", "dl"], writes=["Sgb"])
            ku = bank("w")
            for hh in range(2):
                ps = slice(64 * hh, 64 * hh + 64)
                ka = bank("g")
                P.op("pe", lambda e, ka=ka, ps=ps, cs=cs: e.matmul(pb[ka][:, 0:128], lhsT=kd[ps, cs], rhs=qd[ps, cs], start=True, stop=True),
                     reads=["kd", "qd"], writes=[("pb", ka)])
                P.op("dve", lambda e, ka=ka, hh=hh: e.tensor_tensor(out=ATs[hh][:], in0=pb[ka][:, 0:128], in1=cf[:, mask:mask + 128], op=ALU.mult),
                     reads=[("pb", ka), "cf"], writes=[("ATs", hh)])
                acc = 6
                P.op("pe", lambda e, hh=hh, b4=b4, cs=cs: e.matmul(pb[4 + hh][0:96, cs],
                                                                   lhsT=vt[:, b4, hh * 96:(hh + 1) * 96], rhs=ATs[hh][:], start=True, stop=False),
                     reads=[("ATs", hh), ("vt", b4)], writes=[("pb", 4 + hh)])
                P.op("pe", lambda e, hh=hh, ps=ps, cs=cs: e.matmul(pb[4 + hh][0:96, cs],
                                                                   lhsT=Sgb[ps, :], rhs=qd[ps, cs], start=False, stop=True),
                     reads=["Sgb", "qd"], writes=[("pb", 4 + hh)])
                P.op("pe", lambda e, hh=hh, ps=ps, b4=b4, ai=ai, ku=ku: e.matmul(pb[ku][ps, 0:96], lhsT=ktok[ai][:, ps],
                                                                                rhs=vt[:, b4, hh * 96:(hh + 1) * 96], start=True, stop=True),
                     reads=[("ktok", ai), ("vt", b4)], writes=[("pb", ku)])
            P.op("dve", lambda e, b4=b4, ku=ku: e.scalar_tensor_tensor(out=Sg[:], in0=Sg[:], scalar=dl[:, b4:b4 + 1], in1=pb[ku][:, 0:96],
                                                                      op0=ALU.mult, op1=ALU.add),
                 reads=["Sg", "dl", ("pb", ku)], writes=["Sg"])
            yield
        else:
            Sh, Shb = L["Sh"][h], L["Shb"][h]
            ka = bank("g")
            P.op("pe", lambda e, ka=ka, cs=cs: e.matmul(pb[ka][:, 0:128], lhsT=kd[:, cs], rhs=qd[:, cs], start=True, stop=True),
                 reads=["kd", "qd"], writes=[("pb", ka)])
            P.op("dve", lambda e, ka=ka: e.tensor_tensor(out=ATs[0][:], in0=pb[ka][:, 0:128], in1=cf[:, mask:mask + 128], op=ALU.mult),
                 reads=[("pb", ka), "cf"], writes=[("ATs", 0)])
            P.op("pe", lambda e, b4=b4, cs=cs: e.matmul(pb[4][0:64, cs], lhsT=vt[:, b4, 192 + h * 64:192 + (h + 1) * 64], rhs=ATs[0][:],
                                                        start=True, stop=False),
                 reads=[("ATs", 0), ("vt", b4)], writes=[("pb", 4)])
            for s in range(2):
                blk = 2 * b4 + s
                P.op("dve", lambda e, s=s, b4=b4: e.tensor_scalar(out=L["vz"][s][:], in0=vt[:, b4, 192 + h * 64:192 + (h + 1) * 64],
                                                               scalar1=cf[:, CF_P0 + s:CF_P0 + s + 1], scalar2=None, op0=ALU.mult),
                     reads=[("vt", b4), "cf"], writes=[("vz", s)])
                P.op("dve", lambda e, blk=blk: e.tensor_scalar(out=Shb[:], in0=Sh[:], scalar1=dl[:, blk:blk + 1], scalar2=None, op0=ALU.mult),
                     reads=[("Sh", h), "dl"], writes=[("Shb", h)])
                P.op("pe", lambda e, cs=cs, s=s: e.matmul(pb[4][0:64, cs], lhsT=Shb[:], rhs=L["qz"][s][:, cs], start=False, stop=(s == 1)),
                     reads=[("Shb", h), ("qz", s)], writes=[("pb", 4)])
                ku = bank("g")
                P.op("pe", lambda e, ku=ku, s=s, ai=ai: e.matmul(pb[ku][:, 0:64], lhsT=ktok[ai][:], rhs=L["vz"][s][:], start=True, stop=True),
                     reads=[("ktok", ai), ("vz", s)], writes=[("pb", ku)])
                P.op("dve", lambda e, blk=blk, ku=ku: e.scalar_tensor_tensor(out=Sh[:], in0=Sh[:], scalar=dl[:, blk:blk + 1], in1=pb[ku][:, 0:64],
                                                                            op0=ALU.mult, op1=ALU.add),
                     reads=[("Sh", h), "dl", ("pb", ku)], writes=[("Sh", h)])
                yield
    if kind == "gla":
        for hh in range(2):
            src = pb[4 + hh][0:96, :]
            headnorm(src, [("pb", 4 + hh)], 96, cv[0:96, CV_GLAG:CV_GLAG + 1], mo[mi][0:96, hh, :], [("mo", mi, hh)],
                     extra=L["sgl"][hh][:], extrakeys=[("sgl", hh)])
    else:
        headnorm(pb[4][0:64, :], [("pb", 4)], 64, cv[0:64, CV_HGO:CV_HGO + 1], mo[mi][0:64, 5 + h, :], [("mo", mi, 5 + h)],
                 extra=L["shg"][h][:], extrakeys=[("shg", h)])


def mixer_rows(hh):
    rows = []
    for j in range(2):
        rows.append((96 * j, 96, (2 * hh + j) * 96))
    for j in range(3):
        rows.append((192 + 64 * j, 64, 384 + (3 * hh + j) * 64))
    for j in range(2):
        rows.append((384 + 64 * j, 64, 768 + (2 * hh + j) * 64))
    return rows


_CONSTS = None


def mixer_inputs(p, li, hh, xb):
    global _CONSTS
    if _CONSTS is None:
        _CONSTS = make_consts()
    W = p["w_in"][li]
    offs = np.concatenate([[0], np.cumsum([192, 192, 384, 16, 384, 384, 384, 384, 512, 512, 256, 256])])
    (o_gq, o_gk, o_gv, o_lr, o_gg, o_sq, o_sk, o_sv, o_hq, o_hf, o_hi, o_hg) = offs[:12]
    wc = np.zeros((D, WC), np.float32)

    def put(name, src0, n, dst_off=0):
        off, M = FM_OFF[name]
        wc[:, off + dst_off:off + dst_off + n] = W[:, src0:src0 + n]

    for j in range(2):
        hd = 2 * hh + j
        put("gq", o_gq + hd * 48, 48, 64 * j)
        put("gk", o_gk + hd * 48, 48, 64 * j)
        put(f"gg{j}", o_gg + hd * 96, 96)
        put(f"hq{j}", o_hq + hd * 128, 128)
        put(f"hf{j}", o_hf + hd * 128, 128)
        put(f"hgt{j}", o_hg + hd * 64, 64)
        wc[:, TM_OFF + 192 + 96 * j:TM_OFF + 192 + 96 * (j + 1)] = W[:, o_gv + hd * 96:o_gv + (hd + 1) * 96]
        wc[:, TM_OFF + 384 + 64 * j:TM_OFF + 384 + 64 * (j + 1)] = W[:, o_hi + hd * 64:o_hi + (hd + 1) * 64]
    put("glr", o_lr, 16)
    for j in range(3):
        hd = 3 * hh + j
        put(f"sq{j}", o_sq + hd * 64, 64)
        put(f"sk{j}", o_sk + hd * 64, 64)
        wc[:, TM_OFF + 64 * j:TM_OFF + 64 * (j + 1)] = W[:, o_sv + hd * 64:o_sv + (hd + 1) * 64]
    cvec = np.zeros((128, 16), np.float32)
    wdec = np.zeros((16, 128), np.float32)
    for j in range(2):
        hd = 2 * hh + j
        cvec[64 * j:64 * j + 48, CV_BDEC] = p["gla_b_decay"][li][hd * 48:(hd + 1) * 48]
        wdec[:, 64 * j:64 * j + 48] = p["gla_w_decay"][li][:, hd * 48:(hd + 1) * 48]
        cvec[:, CV_LB + j] = p["hg_lb_logits"][0][hd * 128:(hd + 1) * 128]
        cvec[:, CV_LB + 2 + j] = p["hg_lb_logits"][min(1, p["hg_lb_logits"].shape[0] - 1)][hd * 128:(hd + 1) * 128]
    cvec[0:96, CV_GLAG] = p["gla_out_g"][li]
    cvec[0:64, CV_SBQ] = p["sb_q_g"][li]
    cvec[0:64, CV_SBK] = p["sb_k_g"][li]
    cvec[0:64, CV_SBO] = p["sb_out_g"][li]
    cvec[0:64, CV_HGO] = p["hg_out_g"][li]
    return dict(x=np.ascontiguousarray(xb), w_in=wc,
                g_bc=np.ascontiguousarray(np.broadcast_to(p["norm_mix_g"][li], (128, D))),
                cvec=cvec, wdec=wdec, cf=_CONSTS[0], cb=_CONSTS[1])


_NC_CACHE = {}


def _get(kind, *args):
    key = (kind,) + args
    if key not in _NC_CACHE:
        _NC_CACHE[key] = build_mixer(*args) if kind == "mix" else build_ffn(*args)
    return _NC_CACHE[key]


def kernel_unfused(**inp):
    p = {k: np.asarray(v) for k, v in inp.items()}
    x = np.ascontiguousarray(p["x"], dtype=np.float32)
    B, T, _ = x.shape
    depth = p["w_in"].shape[0]
    ident = np.eye(128, dtype=np.float32).astype(NPBF)
    cores = list(range(8))
    for li in range(depth):
        nc = _get("mix", T, li, "sgh")
        in_maps = [mixer_inputs(p, li, c % 2, x[c // 2]) for c in cores]
        res = run_bass_kernel_spmd(nc, in_maps, core_ids=cores).results
        mT = np.zeros((B, D, T), NPBF)
        for c in cores:
            b, hh = c // 2, c % 2
            m = res[c]["mT"]
            for (r0, n, g0) in mixer_rows(hh):
                mT[b, g0:g0 + n, :] = m[r0:r0 + n, :]
        nc2 = _get("ffn", T // 2)
        in_maps = []
        for c in cores:
            b, th = c // 2, c % 2
            sl = slice(th * (T // 2), (th + 1) * (T // 2))
            in_maps.append(dict(x=np.ascontiguousarray(x[b, sl]), mT=np.ascontiguousarray(mT[b][:, sl]),
                                w_out=p["w_out"][li], w_up=p["w_ffn_up"][li], w_down=p["w_ffn_down"][li],
                                g_bc=np.ascontiguousarray(np.broadcast_to(p["norm_ffn_g"][li], (128, D))), ident=ident))
        res = run_bass_kernel_spmd(nc2, in_maps, core_ids=cores).results
        xn = np.empty_like(x)
        for c in cores:
            b, th = c // 2, c % 2
            xn[b, th * (T // 2):(th + 1) * (T // 2)] = res[c]["y"]
        x = xn
    return x


def build_fused(T, depth=2):
    nc = bass.Bass("TRN2", target_bir_lowering=False)
    x = nc.dram_tensor("x", [T, D], F32, kind="ExternalInput").ap()
    w_in = nc.dram_tensor("w_in", [depth, 2, D, WC], F32, kind="ExternalInput").ap()
    gmix = nc.dram_tensor("gmix", [depth, 128, D], F32, kind="ExternalInput").ap()
    gffn = nc.dram_tensor("gffn", [depth, 128, D], F32, kind="ExternalInput").ap()
    cvec = nc.dram_tensor("cvec", [depth, 2, 128, 16], F32, kind="ExternalInput").ap()
    wdec = nc.dram_tensor("wdec", [depth, 2, 16, 128], F32, kind="ExternalInput").ap()
    cf = nc.dram_tensor("cf", [128, CF_W], F32, kind="ExternalInput").ap()
    cb = nc.dram_tensor("cb", [128, CB_W], BF16, kind="ExternalInput").ap()
    w_out = nc.dram_tensor("w_out", [depth, D, D], F32, kind="ExternalInput").ap()
    w_up = nc.dram_tensor("w_up", [depth, D, 2 * DFF], F32, kind="ExternalInput").ap()
    w_down = nc.dram_tensor("w_down", [depth, DFF, D], F32, kind="ExternalInput").ap()
    y = nc.dram_tensor("y", [T, D], F32, kind="ExternalOutput").ap()
    mTs = nc.dram_tensor("mTs", [D, T], BF16).ap()
    hTs = nc.dram_tensor("hTs", [T // 512, 128, 8 * 512], BF16).ap()
    xs = nc.dram_tensor("xs", [T, D], F32).ap()
    unit = min(UNIT, T)
    for li in range(depth):
        xin = x if li == 0 else xs
        for hh in range(2):
            A = Alloc(nc)
            P = Prog(nc)
            dr = dict(x=xin, w_in=w_in[li, hh], g_bc=gmix[li], cvec=cvec[li, hh], wdec=wdec[li, hh], cf=cf, cb=cb,
                      mT=mTs[hh * 512:(hh + 1) * 512, :])
            dr["hT_out" if hh == 0 else "hT_in"] = hTs
            emit_mixer(nc, A, P, dr, T, li, "sgh")
            P.wait_all("sp", [("mT", t) for t in range(T // 512)] + ([("hTs", t) for t in range(T // 512)] if hh == 0 else []))
            P.emit()
            A.close()
        A = Alloc(nc)
        P = Prog(nc)
        emit_ffn(nc, A, P, xin, mTs, w_out[li], w_up[li], w_down[li], gffn[li], cb[:, CB_IDENT:CB_IDENT + 128],
                 (y if li == depth - 1 else xs), T, unit, T // unit)
        P.wait_all("sp", [("y", u) for u in range(T // unit)])
        P.emit()
        A.close()
    return nc


def fused_inputs(p, b):
    global _CONSTS
    if _CONSTS is None:
        _CONSTS = make_consts()
    depth = p["w_in"].shape[0]
    w_in = np.zeros((depth, 2, D, WC), np.float32)
    cvec = np.zeros((depth, 2, 128, 16), np.float32)
    wdec = np.zeros((depth, 2, 16, 128), np.float32)
    w_out = np.zeros((depth, D, D), np.float32)
    for li in range(depth):
        for hh in range(2):
            m = mixer_inputs(p, li, hh, p["x"][b])
            w_in[li, hh] = m["w_in"]
            cvec[li, hh] = m["cvec"]
            wdec[li, hh] = m["wdec"]
            for (r0, n, g0) in mixer_rows(hh):
                w_out[li, hh * 512 + r0:hh * 512 + r0 + n, :] = p["w_out"][li][g0:g0 + n, :]
    return dict(x=np.ascontiguousarray(p["x"][b]), w_in=w_in,
                gmix=np.ascontiguousarray(np.broadcast_to(p["norm_mix_g"][:, None, :], (depth, 128, D))),
                gffn=np.ascontiguousarray(np.broadcast_to(p["norm_ffn_g"][:, None, :], (depth, 128, D))),
                cvec=cvec, wdec=wdec, cf=_CONSTS[0], cb=_CONSTS[1], w_out=w_out,
                w_up=np.ascontiguousarray(p["w_ffn_up"]), w_down=np.ascontiguousarray(p["w_ffn_down"]))


def kernel(**inp):
    p = {k: np.asarray(v) for k, v in inp.items()}
    p["x"] = np.ascontiguousarray(p["x"], dtype=np.float32)
    B, T, _ = p["x"].shape
    nc = _get_fused(T, p["w_in"].shape[0])
    cores = list(range(8))
    per_b = [fused_inputs(p, b) for b in range(B)]
    in_maps = [per_b[c % B] for c in cores]
    res = run_bass_kernel_spmd(nc, in_maps, core_ids=cores).results
    return np.stack([res[b]["y"] for b in range(B)], axis=0)


def _get_fused(T, depth):
    key = ("fused", T, depth)
    if key not in _NC_CACHE:
        _NC_CACHE[key] = build_fused(T, depth)
    return _NC_CACHE[key]
```
